# Optimizing a Trainium2 kernel written in Bass

```python
import jax, jax.numpy as jnp
from jax import lax
import numpy as np

D_MODEL = 2048
BATCH = 4
SEQ = 4096
DEPTH = 2

N_MIXERS = 2
CHUNK = 128
EPS = 1e-6
GMLP_DFF = 6 * D_MODEL
GMLP_HALF = GMLP_DFF // 2
GMLP_GROUPS = 8
GMLP_GROUP_DIM = GMLP_HALF // GMLP_GROUPS
RET_HEADS = 8
RET_QK_DIM = D_MODEL
RET_V_DIM = 2 * D_MODEL
RET_HEAD_QK = RET_QK_DIM // RET_HEADS
RET_HEAD_V = RET_V_DIM // RET_HEADS
ROPE_BASE = 10000.0
FFN_DIM = 5632
CONV_WIDTH = 3

N_LAYERS_A = (DEPTH + 1) // 2
N_LAYERS_B = DEPTH // 2

kernel_name = "hybrid_gmlp_retention_convffn"


def _rms(xf):
    return xf * lax.rsqrt(jnp.mean(xf * xf, axis=-1, keepdims=True) + EPS)


def rmsnorm(x, g):
    y = _rms(x.astype(jnp.float32)) * g.astype(jnp.float32)
    return y.astype(x.dtype)


def layernorm(x, g, b):
    xf = x.astype(jnp.float32)
    mu = jnp.mean(xf, axis=-1, keepdims=True)
    xc = xf - mu
    var = jnp.mean(xc * xc, axis=-1, keepdims=True)
    y = xc * lax.rsqrt(var + EPS) * g.astype(jnp.float32) + b.astype(jnp.float32)
    return y.astype(x.dtype)


def gmlp_mixer(h, w_in, ln_g, ln_b, w_s, b_s, w_out):
    B, S, _ = h.shape
    n = S // CHUNK
    z = jax.nn.gelu(h @ w_in, approximate=False)
    u, v = jnp.split(z, 2, axis=-1)
    v = layernorm(v, ln_g, ln_b)
    v = v.reshape(B, n, CHUNK, GMLP_GROUPS, GMLP_GROUP_DIM)
    mask = jnp.tril(jnp.ones((CHUNK, CHUNK), dtype=bool))
    ws = jnp.where(mask[None], w_s, 0).astype(v.dtype)
    mixed = jnp.einsum('gts,bnsgd->bntgd', ws, v) + b_s.T.astype(v.dtype)[None, None, :, :, None]
    gated = u * mixed.reshape(B, S, GMLP_HALF)
    return gated @ w_out


def rotate_every_two(x):
    x1 = x[..., ::2]
    x2 = x[..., 1::2]
    return jnp.stack((-x2, x1), axis=-1).reshape(x.shape)


def retention_mixer(h, positions, w_in, w_out):
    B, S, _ = h.shape
    n = S // CHUNK
    dt = h.dtype
    proj = h @ w_in
    q, k, v, g = jnp.split(proj, [RET_QK_DIM, 2 * RET_QK_DIM, 2 * RET_QK_DIM + RET_V_DIM], axis=-1)
    q = q.reshape(B, S, RET_HEADS, RET_HEAD_QK)
    k = k.reshape(B, S, RET_HEADS, RET_HEAD_QK)
    v = v.reshape(B, S, RET_HEADS, RET_HEAD_V)
    inv_freq = 1.0 / (ROPE_BASE ** jnp.linspace(0.0, 1.0, RET_HEAD_QK // 2, dtype=jnp.float32))
    inv_freq = jnp.repeat(inv_freq, 2)
    ang = positions.astype(jnp.float32)[..., None] * inv_freq
    cos = jnp.cos(ang)[:, :, None, :]
    sin = jnp.sin(ang)[:, :, None, :]
    q = (q * cos + rotate_every_two(q) * sin).astype(dt)
    k = ((k * cos + rotate_every_two(k) * sin) * (RET_HEAD_QK ** -0.5)).astype(dt)
    log_gamma = jnp.log1p(-jnp.exp2(-5.0 - jnp.arange(RET_HEADS, dtype=jnp.float32)))
    idx = jnp.arange(CHUNK, dtype=jnp.float32)
    rel = idx[:, None] - idx[None, :]
    decay_mask = jnp.where(rel[None] >= 0, jnp.exp(jnp.maximum(rel, 0.0)[None] * log_gamma[:, None, None]), 0.0)
    qc = q.reshape(B, n, CHUNK, RET_HEADS, RET_HEAD_QK)
    kc = k.reshape(B, n, CHUNK, RET_HEADS, RET_HEAD_QK)
    vc = v.reshape(B, n, CHUNK, RET_HEADS, RET_HEAD_V)
    scores = jnp.einsum('bnchd,bnshd->bnhcs', qc, kc) * decay_mask.astype(dt)
    intra = jnp.einsum('bnhcs,bnshe->bnche', scores, vc)
    q_decay = jnp.exp((idx[:, None] + 1.0) * log_gamma[None, :])
    k_decay = jnp.exp((CHUNK - 1.0 - idx)[:, None] * log_gamma[None, :])
    chunk_decay = jnp.exp(CHUNK * log_gamma)
    qd = qc * q_decay[:, :, None].astype(dt)
    kd = kc * k_decay[:, :, None].astype(dt)
    xs = (jnp.moveaxis(qd, 1, 0), jnp.moveaxis(kd, 1, 0), jnp.moveaxis(vc, 1, 0))

    def step(state, inp):
        qi, ki, vi = inp
        cross_i = jnp.einsum('bchd,bhde->bche', qi.astype(jnp.float32), state)
        state = state * chunk_decay[None, :, None, None] + jnp.einsum(
            'bchd,bche->bhde', ki.astype(jnp.float32), vi.astype(jnp.float32))
        return state, cross_i.astype(dt)

    state0 = jnp.zeros((B, RET_HEADS, RET_HEAD_QK, RET_HEAD_V), jnp.float32)
    _, cross = lax.scan(step, state0, xs)
    o = intra + jnp.moveaxis(cross, 0, 1)
    o = o.reshape(B, S, RET_HEADS, RET_HEAD_V)
    o = _rms(o.astype(jnp.float32)).astype(dt).reshape(B, S, RET_V_DIM)
    return (jax.nn.silu(g) * o) @ w_out


def conv_ffn(h, w_up, conv_w, conv_b, w_down):
    S = h.shape[1]
    z = h @ w_up
    zp = jnp.pad(z, ((0, 0), (CONV_WIDTH - 1, 0), (0, 0)))
    zc = conv_b.astype(z.dtype)
    for tap in range(CONV_WIDTH):
        zc = zc + conv_w[tap].astype(z.dtype) * zp[:, tap:tap + S]
    gate, up = jnp.split(zc, 2, axis=-1)
    return (jax.nn.silu(gate) * up) @ w_down


def setup_inputs(seed: int = 0) -> dict:
    key = jax.random.key(seed)
    ks = jax.random.split(key, 20)
    f32 = jnp.float32

    def nrm(k, shape, scale):
        return jax.random.normal(k, shape, f32) * scale

    def gain(k, shape):
        return 1.0 + 0.02 * jax.random.normal(k, shape, f32)

    x = jax.random.normal(ks[0], (BATCH, SEQ, D_MODEL), f32)
    positions = jnp.broadcast_to(jnp.arange(SEQ, dtype=jnp.int32), (BATCH, SEQ))
    return {
        "x": x,
        "positions": positions,
        "mix_pre_g": gain(ks[1], (DEPTH, D_MODEL)),
        "mix_post_g": gain(ks[2], (DEPTH, D_MODEL)),
        "gmlp_w_in": nrm(ks[3], (N_LAYERS_A, D_MODEL, GMLP_DFF), D_MODEL ** -0.5),
        "gmlp_ln_g": gain(ks[4], (N_LAYERS_A, GMLP_HALF)),
        "gmlp_ln_b": nrm(ks[5], (N_LAYERS_A, GMLP_HALF), 0.02),
        "gmlp_w_s": nrm(ks[6], (N_LAYERS_A, GMLP_GROUPS, CHUNK, CHUNK), CHUNK ** -0.5),
        "gmlp_b_s": 1.0 + nrm(ks[7], (N_LAYERS_A, GMLP_GROUPS, CHUNK), 0.1),
        "gmlp_w_out": nrm(ks[8], (N_LAYERS_A, GMLP_HALF, D_MODEL), GMLP_HALF ** -0.5),
        "ret_w_in": nrm(ks[9], (N_LAYERS_B, D_MODEL, 2 * RET_QK_DIM + 2 * RET_V_DIM), D_MODEL ** -0.5),
        "ret_w_out": nrm(ks[10], (N_LAYERS_B, RET_V_DIM, D_MODEL), RET_V_DIM ** -0.5),
        "ffn_pre_g": gain(ks[11], (DEPTH, D_MODEL)),
        "ffn_post_g": gain(ks[12], (DEPTH, D_MODEL)),
        "ffn_w_up": nrm(ks[13], (DEPTH, D_MODEL, 2 * FFN_DIM), D_MODEL ** -0.5),
        "ffn_conv_w": nrm(ks[14], (DEPTH, CONV_WIDTH, 2 * FFN_DIM), CONV_WIDTH ** -0.5),
        "ffn_conv_b": nrm(ks[15], (DEPTH, 2 * FFN_DIM), 0.02),
        "ffn_w_down": nrm(ks[16], (DEPTH, FFN_DIM, D_MODEL), FFN_DIM ** -0.5),
    }


def reference(x, positions, mix_pre_g, mix_post_g, gmlp_w_in, gmlp_ln_g, gmlp_ln_b, gmlp_w_s,
              gmlp_b_s, gmlp_w_out, ret_w_in, ret_w_out, ffn_pre_g, ffn_post_g, ffn_w_up,
              ffn_conv_w, ffn_conv_b, ffn_w_down):
    for i in range(DEPTH):
        j = i // N_MIXERS
        hn = rmsnorm(x, mix_pre_g[i])
        if i % N_MIXERS == 0:
            y = gmlp_mixer(hn, gmlp_w_in[j], gmlp_ln_g[j], gmlp_ln_b[j], gmlp_w_s[j],
                           gmlp_b_s[j], gmlp_w_out[j])
        else:
            y = retention_mixer(hn, positions, ret_w_in[j], ret_w_out[j])
        x = x + rmsnorm(y, mix_post_g[i])
        hn = rmsnorm(x, ffn_pre_g[i])
        y = conv_ffn(hn, ffn_w_up[i], ffn_conv_w[i], ffn_conv_b[i], ffn_w_down[i])
        x = x + rmsnorm(y, ffn_post_g[i])
    return x
```

```python
import math
import numpy as np
import concourse.bass as bass
import concourse.mybir as mybir
from concourse.bass_utils import run_bass_kernel_spmd

F32, BF16, I32, U8 = mybir.dt.float32, mybir.dt.bfloat16, mybir.dt.int32, mybir.dt.uint8
AF = mybir.ActivationFunctionType
ALU = mybir.AluOpType
AX = mybir.AxisListType

D = 2048
T = 2048
NCORE = 8
EPS = 1e-6
GF = 6144
FF = 5632
KB = 1024
SHARD_W = True


class Buf:
    def __init__(self, name):
        self.name = name
        self.w = None
        self.r = {}
        self.dsem = None
        self.dcnt = 0


class K:
    ENG = ["pe", "act", "dve", "pool", "sp"]

    def __init__(self, nc):
        self.nc = nc
        self.prog = {e: [] for e in self.ENG}
        self.sem = {e: nc.alloc_semaphore(name="pg_" + e) for e in self.ENG}
        self.cnt = {e: 0 for e in self.ENG}
        self.waited = {e: {} for e in self.ENG}
        self.sempool = []
        self.dbufs = []
        self.extra_toks = []

    def _wait(self, eng, s, v):
        if self.waited[eng].get(s, 0) >= v:
            return
        self.waited[eng][s] = v
        self.prog[eng].append(lambda e, s=s, v=v: e.wait_ge(s, v))

    def _deps(self, eng, reads, writes):
        deps = {}

        def add(tok):
            if tok is None:
                return
            s, v = tok
            if deps.get(s, 0) < v:
                deps[s] = v
        for b in reads:
            add(b.w)
        for b in writes:
            add(b.w)
            for s, v in b.r.items():
                add((s, v))
        own = self.sem[eng]
        for s, v in deps.items():
            if eng == "pe" and s is own:
                continue
            self._wait(eng, s, v)

    def _mark(self, tok, reads, writes):
        for b in reads:
            if b.r.get(tok[0], 0) < tok[1]:
                b.r[tok[0]] = tok[1]
        for b in writes:
            b.w = tok
            b.r = {}

    def emit(self, eng, fn, reads=(), writes=(), inc=True):
        self._deps(eng, reads, writes)
        s = self.sem[eng]
        tok = (s, self.cnt[eng] + 1)
        if inc:
            self.cnt[eng] += 1
            self.prog[eng].append(lambda e, fn=fn, s=s: fn(e).then_inc(s, 1))
        else:
            self.prog[eng].append(lambda e, fn=fn: fn(e))
        self._mark(tok, reads, writes)

    def _getsem(self, owner):
        if owner.dsem is None:
            if self.sempool:
                owner.dsem, owner.dcnt = self.sempool.pop()
            else:
                owner.dsem = self.nc.alloc_semaphore(name="d_" + owner.name)
                owner.dcnt = 0
            self.dbufs.append(owner)

    def dma(self, eng, out_ap, in_ap, owner, reads=(), writes=()):
        self._deps(eng, reads, writes)
        self._getsem(owner)
        owner.dcnt += 16
        tok = (owner.dsem, owner.dcnt)
        self.prog[eng].append(
            lambda e, o=out_ap, i=in_ap, s=owner.dsem: e.dma_start(out=o, in_=i).then_inc(s, 16))
        self._mark(tok, reads, writes)
        return tok

    def collective(self, src_ap, dst_ap, groups, reads, writes):
        eng = "pool"
        self._deps(eng, reads, writes)
        sem = self.nc.alloc_semaphore(name="cc%d" % len(self.extra_toks))
        tok = (sem, 1)
        self.extra_toks.append(tok)
        self.prog[eng].append(
            lambda e, a=src_ap, b=dst_ap, g=groups, s=sem: e.collective_compute(
                "AllGather", ALU.bypass, replica_groups=g, ins=[a], outs=[b]).then_inc(s, 1))
        self._mark(tok, reads, writes)
        return tok

    def barrier(self):
        toks = [(self.sem[o], self.cnt[o]) for o in self.ENG if self.cnt[o] > 0]
        toks += [(b.dsem, b.dcnt) for b in self.dbufs if b.dcnt > 0]
        toks += self.extra_toks
        for e in self.ENG:
            for s, v in toks:
                if s is self.sem[e]:
                    continue
                self._wait(e, s, v)
        for b in self.dbufs:
            self.sempool.append((b.dsem, b.dcnt))
            b.dsem = None
        self.dbufs = []

    def finish(self):
        nc = self.nc
        with nc.Block() as block:
            @block.tensor
            def _(e):
                for f in self.prog["pe"]:
                    f(e)

            @block.scalar
            def _(e):
                for f in self.prog["act"]:
                    f(e)

            @block.vector
            def _(e):
                for f in self.prog["dve"]:
                    f(e)

            @block.gpsimd
            def _(e):
                for f in self.prog["pool"]:
                    f(e)

            @block.sync
            def _(e):
                for f in self.prog["sp"]:
                    f(e)


class Prog:
    def __init__(self, debug=False, ntiles=99):
        self.debug = debug
        self.ntiles = ntiles
        nc = self.nc = bass.Bass("TRN2", target_bir_lowering=False)
        self.k = K(nc)
        dt = nc.dram_tensor
        self.x_in = dt("x", [T, D], F32, kind="ExternalInput")
        self.pos = dt("pos", [16, 128], I32, kind="ExternalInput")
        self.flag = dt("sel", [128, 8], F32, kind="ExternalInput")
        self.norm_g = dt("norm_g", [8, D], F32, kind="ExternalInput")
        self.g_ln = dt("g_ln", [2, GF], F32, kind="ExternalInput")
        self.g_ws = dt("g_ws", [8 * 128, 128], F32, kind="ExternalInput")
        self.g_bs = dt("g_bs", [8, 128], F32, kind="ExternalInput")
        self.f_cw = [dt("f_cw%d" % i, [3 * 88, 128], F32, kind="ExternalInput") for i in range(2)]
        self.f_cb = [dt("f_cb%d" % i, [88, 128], F32, kind="ExternalInput") for i in range(2)]
        self.wspecs = [("g_w_in", D, 2 * GF), ("g_w_out", GF, D), ("f_w_up0", D, 2 * FF), ("f_w_dn0", FF, D),
                       ("r_w_in", D, 12288), ("r_w_out", 4096, D), ("f_w_up1", D, 2 * FF), ("f_w_dn1", FF, D)]
        self.W = {}
        self.Wb = {}
        self.Wsh = {}
        self.Wbn = {}
        for name, R, C in self.wspecs:
            if SHARD_W:
                self.Wsh[name] = dt(name, [R // NCORE, C], F32, kind="ExternalInput")
                self.Wbn[name] = dt(name + "_bn", [R // NCORE, C], F32)
                self.W[name] = dt(name + "_full", [R, C], F32)
            else:
                self.W[name] = dt(name, [R, C], F32, kind="ExternalInput")
            self.Wb[name] = Buf("w_" + name)
        self.g_w_in, self.g_w_out = self.W["g_w_in"], self.W["g_w_out"]
        self.r_w_in, self.r_w_out = self.W["r_w_in"], self.W["r_w_out"]
        self.f_w_up = [self.W["f_w_up0"], self.W["f_w_up1"]]
        self.f_w_dn = [self.W["f_w_dn0"], self.W["f_w_dn1"]]
        self.out = dt("out", [T, D], F32, kind="ExternalOutput")
        self.xs = dt("xs_scr", [T, D], F32)
        self.hal_src = dt("hal_src", [2, D], F32)
        self.hal_dst = dt("hal_dst", [16, D], F32)
        self.st = dt("st_scr", [128, 8192], F32)
        self.st2 = dt("st2_scr", [1024, 8192], F32)
        self.dumps = []
        self.b_xs = [Buf("xs%d" % c) for c in range(16)]
        self.b_out = [Buf("out%d" % c) for c in range(16)]
        self.b_halsrc, self.b_haldst = Buf("halsrc"), Buf("haldst")
        self.b_st = [Buf("st%d" % h) for h in range(8)]
        self.b_st2 = Buf("st2")
        self.b_d2d = Buf("d2d")
        total = 206 * KB
        nc.alloc_sbuf_tensor("arena", [128, total], U8)
        self.abase = nc.sbuf_base - total
        self.atotal = total
        self.cur = 0
        self.ps = [nc.alloc_psum_tensor("ps%d" % i, [128, 512], F32) for i in range(8)]
        self.b_ps = [Buf("ps%d" % i) for i in range(8)]
        self.psi = 0
        self.nalloc = 0

    def alloc(self, name, shape, dtype):
        nbytes = int(np.prod(shape[1:])) * (2 if dtype == BF16 else 4)
        nbytes = (nbytes + 63) // 64 * 64
        assert self.cur + nbytes <= self.atotal, (name, self.cur, nbytes)
        self.nalloc += 1
        t = self.nc.alloc_sbuf_tensor_at("%s_%d" % (name, self.nalloc), list(shape), dtype,
                                         offset=self.abase + self.cur)
        self.cur += nbytes
        return t

    def bank(self):
        i = self.psi
        self.psi = (self.psi + 1) % 8
        return self.ps[i], self.b_ps[i]

    def mm(self, ps, bps, lhsT, rhs, start, stop, reads, inc=None):
        self.k.emit("pe", lambda e: e.matmul(ps, lhsT, rhs, start=start, stop=stop),
                    reads=reads, writes=[bps], inc=(stop if inc is None else inc))

    def tr(self, ps, bps, in_, ident, reads, inc=True):
        self.k.emit("pe", lambda e: e.transpose(ps, in_, ident), reads=reads, writes=[bps], inc=inc)

    def act(self, out, in_, func, reads, writes, scale=None):
        if scale is None:
            self.k.emit("act", lambda e: e.activation(out=out, in_=in_, func=func), reads=reads, writes=writes)
        else:
            self.k.emit("act", lambda e: e.activation(out=out, in_=in_, func=func, scale=scale),
                        reads=reads, writes=writes)

    def ts(self, out, in0, s1, s2, op0, op1, reads, writes, eng="dve"):
        if op1 is None:
            self.k.emit(eng, lambda e: e.tensor_scalar(out=out, in0=in0, scalar1=s1, scalar2=None, op0=op0),
                        reads=reads, writes=writes)
        else:
            self.k.emit(eng, lambda e: e.tensor_scalar(out=out, in0=in0, scalar1=s1, scalar2=s2, op0=op0, op1=op1),
                        reads=reads, writes=writes)

    def tt(self, out, in0, in1, op, reads, writes, eng="dve"):
        self.k.emit(eng, lambda e: e.tensor_tensor(out=out, in0=in0, in1=in1, op=op), reads=reads, writes=writes)

    def stt(self, out, in0, scalar, in1, op0, op1, reads, writes):
        self.k.emit("dve", lambda e: e.scalar_tensor_tensor(out=out, in0=in0, scalar=scalar, in1=in1, op0=op0, op1=op1),
                    reads=reads, writes=writes)

    def red(self, out, in_, reads, writes):
        self.k.emit("dve", lambda e: e.tensor_reduce(out=out, in_=in_, axis=AX.X, op=ALU.add),
                    reads=reads, writes=writes)

    def bcast(self, handle, off, n, parts=128):
        return bass.AP(handle, off, [[0, parts], [1, n]])

    def setup_common(self):
        k = self.k
        self.ident = self.alloc("ident", [128, 128], F32)
        self.b_ident = Buf("ident")
        self.preg = self.alloc("preg", [128, 4, 16], F32)
        self.b_preg = Buf("preg")
        self.flag_sb = self.alloc("flag", [128, 8], F32)
        self.b_flag = Buf("flagsb")
        self.hnT = self.alloc("hnT", [128, 16, 512], BF16)
        self.b_hnT = Buf("hnT")
        self.big = self.alloc("big32", [128, 4, 2048], F32)
        self.b_big = [Buf("big%d" % i) for i in range(4)]
        self.xt = self.alloc("xt", [128, 2048], F32)
        self.b_xt = Buf("xt")
        self.scr = self.alloc("scr", [128, 2048], BF16)
        self.b_scr = Buf("scr")
        self.gb = self.alloc("gb", [128, 2048], F32)
        self.b_gb = Buf("gb")
        self.sm = self.alloc("small", [128, 16], F32)
        self.b_sm = Buf("small")
        self.region0 = self.cur
        k.emit("pool", lambda e: e.memset(self.ident[:], 1.0), writes=[self.b_ident])
        k.emit("pool", lambda e: e.affine_select(out=self.ident[:], in_=self.ident[:], pattern=[[-1, 128]],
                                                 compare_op=ALU.is_equal, fill=0.0, base=0, channel_multiplier=1),
               reads=[self.b_ident], writes=[self.b_ident])
        k.dma("sp", self.flag_sb[:], self.flag.ap(), owner=self.b_flag, writes=[self.b_flag])
        stage = self.xt
        for i, row in enumerate((0, 2, 4, 6)):
            src = bass.AP(self.norm_g, row * D, [[128, 16], [1, 128]])
            k.dma("sp", stage[0:16, 0:128], src, owner=self.b_xt, writes=[self.b_xt])
            ps, bps = self.bank()
            self.tr(ps[:, 0:16], bps, stage[0:16, 0:128], self.ident[0:16, 0:16], reads=[self.b_xt, self.b_ident])
            k.emit("dve", lambda e, ps=ps, i=i: e.tensor_copy(out=self.preg[:, i, :], in_=ps[:, 0:16]),
                   reads=[bps], writes=[self.b_preg])

    def gather_weights(self):
        if not SHARD_W:
            return
        k = self.k
        for name, R, C in self.wspecs:
            bb = Buf("bn_" + name)
            rows = R // NCORE
            step = max(1, (2 << 20) // (C * 4))
            for r0 in range(0, rows, step):
                r1 = min(rows, r0 + step)
                k.dma("sp", self.Wbn[name][r0:r1, :], self.Wsh[name][r0:r1, :], owner=bb, writes=[bb])
            k.collective(self.Wbn[name].ap().opt(), self.W[name].ap().opt(), [list(range(NCORE))],
                         reads=[bb], writes=[self.Wb[name]])

    def load_fm(self, dst_ap, bdst, src_rows_ap, R):
        k = self.k
        k.dma("sp", self.xt[0:R, 0:128], src_rows_ap, owner=self.b_xt, writes=[self.b_xt])
        ps, bps = self.bank()
        self.tr(ps[:, 0:R], bps, self.xt[0:R, 0:128], self.ident[0:R, 0:R], reads=[self.b_xt, self.b_ident])
        k.emit("dve", lambda e: e.tensor_copy(out=dst_ap, in_=ps[:, 0:R]), reads=[bps], writes=[bdst])

    def new_region(self, nslots):
        self.cur = self.region0
        self.slots = [self.alloc("slot", [128, 16, 512], BF16) for _ in range(nslots)]
        self.b_slots = [Buf("slot%d" % i) for i in range(nslots)]
        self.si = 0

    def slot(self):
        i = self.si
        self.si = (self.si + 1) % len(self.slots)
        return self.slots[i], self.b_slots[i]

    def load_w(self, sl, bsl, w, r0, nk, c0, ncols, coff=0):
        src = w[r0:r0 + nk * 128, c0:c0 + ncols].rearrange("(k p) n -> p k n", p=128)
        wb = [b for n_, b in self.Wb.items() if self.W[n_] is w]
        self.k.dma("pool", sl[:, 0:nk, coff:coff + ncols], src, owner=bsl, reads=wb, writes=[bsl])

    def rsqrt_ap(self, a, buf, scale, np_=128):
        self.ts(a, a, scale, EPS, ALU.mult, ALU.add, reads=[buf], writes=[buf])
        self.act(a, a, AF.Sqrt, reads=[buf], writes=[buf])
        self.k.emit("dve", lambda e: e.reciprocal(out=a, in_=a), reads=[buf], writes=[buf])

    def rstd_from_ss(self, col, n):
        self.rsqrt_ap(self.sm[:, col:col + 1], self.b_sm, 1.0 / n)

    def prenorm(self, src, bsrc, chunks, gi):
        k = self.k
        n = len(chunks)
        for ci, c in enumerate(chunks):
            k.dma("sp", self.xt[:], src[c * 128:(c + 1) * 128, :], owner=self.b_xt,
                  reads=([bsrc[c]] if bsrc else []), writes=[self.b_xt])
            self.tt(self.scr[:], self.xt[:], self.xt[:], ALU.mult, reads=[self.b_xt], writes=[self.b_scr])
            self.red(self.sm[:, 0:1], self.scr[:], reads=[self.b_scr], writes=[self.b_sm])
            self.rstd_from_ss(0, D)
            self.ts(self.big[:, ci, :], self.xt[:], self.sm[:, 0:1], None, ALU.mult, None,
                    reads=[self.b_xt, self.b_sm], writes=[self.b_big[ci]])
        for kk in range(16):
            ps, bps = self.bank()
            for ci in range(n):
                self.tr(ps[:, ci * 128:(ci + 1) * 128], bps, self.big[:, ci, kk * 128:(kk + 1) * 128], self.ident[:],
                        reads=[self.b_big[ci], self.b_ident], inc=(ci == n - 1))
            self.ts(self.hnT[:, kk, 0:n * 128], ps[:, 0:n * 128], self.preg[:, gi, kk:kk + 1], None, ALU.mult, None,
                    reads=[bps, self.b_preg], writes=[self.b_hnT])

    def outproj(self, actT, bact, KC, w_out, grow, src, bsrc, dst, bdst, chunks):
        k = self.k
        n = len(chunks)
        bacts = list(bact) if isinstance(bact, (list, tuple)) else [bact]
        k.dma("sp", self.gb[:], self.bcast(self.norm_g, grow * D, D), owner=self.b_gb, writes=[self.b_gb])
        for nb in range(4):
            banks = [self.bank() for _ in range(n)]
            for kg in range(0, KC, 16):
                kn = min(16, KC - kg)
                sl, bsl = self.slot()
                self.load_w(sl, bsl, w_out, kg * 128, kn, nb * 512, 512)
                for ci in range(n):
                    ps, bps = banks[ci]
                    for kk in range(kn):
                        self.mm(ps[:], bps, actT[:, kg + kk, ci * 128:(ci + 1) * 128], sl[:, kk, :],
                                start=(kg + kk == 0), stop=(kg + kk == KC - 1), reads=bacts + [bsl],
                                inc=(kk == kn - 1))
            for ci in range(n):
                ps, bps = banks[ci]
                self.act(self.big[:, ci, nb * 512:(nb + 1) * 512], ps[:], AF.Copy, reads=[bps], writes=[self.b_big[ci]])
        for ci, c in enumerate(chunks):
            y = self.big[:, ci, :]
            self.tt(self.scr[:], y, y, ALU.mult, reads=[self.b_big[ci]], writes=[self.b_scr])
            self.red(self.sm[:, 0:1], self.scr[:], reads=[self.b_scr], writes=[self.b_sm])
            self.rstd_from_ss(0, D)
            self.stt(y, y, self.sm[:, 0:1], self.gb[:], ALU.mult, ALU.mult,
                     reads=[self.b_big[ci], self.b_sm, self.b_gb], writes=[self.b_big[ci]])
            k.dma("sp", self.xt[:], src[c * 128:(c + 1) * 128, :], owner=self.b_xt,
                  reads=([bsrc[c]] if bsrc else []), writes=[self.b_xt])
            self.tt(self.xt[:], self.xt[:], y, ALU.add, reads=[self.b_xt, self.b_big[ci]], writes=[self.b_xt])
            k.dma("sp", dst[c * 128:(c + 1) * 128, :], self.xt[:], owner=self.b_xt,
                  reads=[self.b_xt], writes=[bdst[c]])

    def gmlp(self, src, bsrc, dst, bdst):
        k = self.k
        NCH = 4
        self.new_region(4)
        off = self.cur
        vg = self.alloc("vg", [128, 12, NCH * 512], BF16)
        gT = self.nc.alloc_sbuf_tensor_at("gTalias", [128, 48, 512], BF16, offset=self.abase + off)
        b_vg = [Buf("vg%d" % i) for i in range(12)]
        lnp = self.alloc("lnp", [128, 2, 512], F32)
        b_lnp = Buf("lnp")
        tmp = self.alloc("tmp", [128, 512], F32)
        b_tmp = Buf("tmp")
        ub = self.alloc("ub", [128, 512], F32)
        b_ub = Buf("ub")
        gt = self.alloc("gt", [128, NCH, 512], F32)
        b_gt = [Buf("gt%d" % i) for i in range(NCH)]
        st = self.alloc("bnst", [128, 12, 6], F32)
        b_st = Buf("bnst")
        mv = self.alloc("mv", [128, NCH, 2], F32)
        b_mv = Buf("mv")
        wsT = self.alloc("wsT", [128, 8, 128], BF16)
        b_wsT = Buf("wsT")
        wtmp = self.alloc("wtmp", [128, 128], F32)
        b_wtmp = Buf("wtmp")
        bs_tm = self.alloc("bs_tm", [128, 8], F32)
        b_bs = Buf("bs_tm")

        def vblk(nb, ci, c0=0, n=512):
            return vg[:, nb, ci * 512 + c0:ci * 512 + c0 + n]
        for g in range(8):
            k.dma("sp", self.xt[:, 0:128], self.g_ws[g * 128:(g + 1) * 128, :], owner=self.b_xt, writes=[self.b_xt])
            ps, bps = self.bank()
            self.tr(ps[:, 0:128], bps, self.xt[:, 0:128], self.ident[:], reads=[self.b_xt, self.b_ident])
            k.emit("act", lambda e, ps=ps: e.activation(out=wtmp[:], in_=ps[:, 0:128], func=AF.Copy),
                   reads=[bps], writes=[b_wtmp])
            k.emit("pool", lambda e: e.affine_select(out=wtmp[:], in_=wtmp[:], pattern=[[1, 128]],
                                                     compare_op=ALU.is_ge, fill=0.0, base=0, channel_multiplier=-1),
                   reads=[b_wtmp], writes=[b_wtmp])
            k.emit("dve", lambda e, g=g: e.tensor_copy(out=wsT[:, g, :], in_=wtmp[:]), reads=[b_wtmp], writes=[b_wsT])
        self.load_fm(bs_tm[:, :], b_bs, self.g_bs[0:8, :], 8)
        for tile in range(min(self.ntiles, T // (NCH * 128))):
            chunks = [tile * NCH + i for i in range(NCH)]
            self.prenorm(src, bsrc, chunks, 0)
            for nb in range(12):
                sl, bsl = self.slot()
                self.load_w(sl, bsl, self.g_w_in, 0, 16, GF + nb * 512, 512)
                for ci in range(NCH):
                    ps, bps = self.bank()
                    for kk in range(16):
                        self.mm(ps[:], bps, self.hnT[:, kk, ci * 128:(ci + 1) * 128], sl[:, kk, :],
                                start=(kk == 0), stop=(kk == 15), reads=[self.b_hnT, bsl])
                    self.act(vblk(nb, ci), ps[:], AF.Gelu, reads=[bps], writes=[b_vg[nb]])
            for ci in range(NCH):
                for nb in range(12):
                    k.emit("dve", lambda e, ci=ci, nb=nb: e.bn_stats(out=st[:, nb, :], in_=vblk(nb, ci)),
                           reads=[b_vg[nb]], writes=[b_st])
                k.emit("dve", lambda e, ci=ci: e.bn_aggr(out=mv[:, ci, :], in_=st[:].rearrange("p a b -> p (a b)")),
                       reads=[b_st], writes=[b_mv])
                self.rsqrt_ap(mv[:, ci, 1:2], b_mv, 1.0)
            for nb in range(12):
                k.dma("sp", lnp[:, 0, :], self.bcast(self.g_ln, nb * 512, 512), owner=b_lnp, writes=[b_lnp])
                k.dma("sp", lnp[:, 1, :], self.bcast(self.g_ln, GF + nb * 512, 512), owner=b_lnp, writes=[b_lnp])
                for ci in range(NCH):
                    vb = vblk(nb, ci)
                    self.ts(tmp[:], vb, mv[:, ci, 0:1], mv[:, ci, 1:2], ALU.subtract, ALU.mult,
                            reads=[b_vg[nb], b_mv], writes=[b_tmp])
                    self.tt(tmp[:], tmp[:], lnp[:, 0, :], ALU.mult, reads=[b_tmp, b_lnp], writes=[b_tmp])
                    self.tt(vb, tmp[:], lnp[:, 1, :], ALU.add, reads=[b_tmp, b_lnp], writes=[b_vg[nb]])
            for nb in range(12):
                sl, bsl = self.slot()
                self.load_w(sl, bsl, self.g_w_in, 0, 16, nb * 512, 512)
                for ci in range(NCH):
                    ps, bps = self.bank()
                    for kk in range(16):
                        self.mm(ps[:], bps, self.hnT[:, kk, ci * 128:(ci + 1) * 128], sl[:, kk, :],
                                start=(kk == 0), stop=(kk == 15), reads=[self.b_hnT, bsl])
                    self.act(ub[:], ps[:], AF.Gelu, reads=[bps], writes=[b_ub])
                    pm, bpm = self.bank()
                    for hh in range(2):
                        g = (nb * 512 + hh * 256) // 768
                        self.mm(pm[:, hh * 256:(hh + 1) * 256], bpm, wsT[:, g, :], vblk(nb, ci, hh * 256, 256),
                                start=True, stop=True, reads=[b_wsT, b_vg[nb]], inc=(hh == 1))
                    for hh in range(2):
                        g = (nb * 512 + hh * 256) // 768
                        self.stt(gt[:, ci, hh * 256:(hh + 1) * 256], pm[:, hh * 256:(hh + 1) * 256], bs_tm[:, g:g + 1],
                                 ub[:, hh * 256:(hh + 1) * 256], ALU.add, ALU.mult,
                                 reads=[bpm, b_bs, b_ub], writes=[b_gt[ci]])
                for ci in range(NCH):
                    pt, bpt = self.bank()
                    for j in range(4):
                        self.tr(pt[:, j * 128:(j + 1) * 128], bpt, gt[:, ci, j * 128:(j + 1) * 128], self.ident[:],
                                reads=[b_gt[ci], self.b_ident], inc=(j == 3))
                    k.emit("act", lambda e, pt=pt, nb=nb, ci=ci: e.activation(
                        out=gT[:, nb * 4:(nb + 1) * 4, ci * 128:(ci + 1) * 128],
                        in_=pt[:].rearrange("p (j t) -> p j t", j=4), func=AF.Copy),
                        reads=[bpt], writes=[b_vg[nb]])
            self.outproj(gT, b_vg, 48, self.g_w_out, 1, src, bsrc, dst, bdst, chunks)
        k.barrier()

    def ffn(self, layer, src, bsrc, dst, bdst, gi, grow):
        k = self.k
        NCH = 4
        self.new_region(4)
        w_up, w_dn = self.f_w_up[layer], self.f_w_dn[layer]
        actT = self.alloc("actT", [128, 44, 512], BF16)
        b_actT = Buf("actT")
        zs = [self.alloc("zs", [128, 2, 514], F32) for _ in range(2)]
        b_zs = [Buf("zs0"), Buf("zs1")]
        acc = [self.alloc("acc", [128, 2, 512], F32) for _ in range(2)]
        b_acc = [Buf("acc0"), Buf("acc1")]
        sg = self.alloc("sg", [128, 512], F32)
        b_sg = Buf("sg")
        zh = self.alloc("zhalo", [128, 88, 2], F32)
        b_zh = Buf("zhalo")
        cw = self.alloc("cw", [128, 3, 88], F32)
        b_cw = Buf("cw")
        cb = self.alloc("cb", [128, 88], F32)
        b_cb = Buf("cb")
        hh_tm = self.big[:, 0, :]
        b_hh = self.b_big[0]
        hhT = self.alloc("hal_T", [128, 16, 2], BF16)
        b_hhT = Buf("hal_T")
        for tap in range(3):
            self.load_fm(cw[:, tap, :], b_cw, self.f_cw[layer][tap * 88:(tap + 1) * 88, :], 88)
        self.load_fm(cb[:, :], b_cb, self.f_cb[layer][0:88, :], 88)
        import os
        NOHALO = os.environ.get("FFN_NOHALO") == "1"
        k.emit("pool", lambda e: e.memset(zh[:], 0.0), writes=[b_zh])
        if not NOHALO:
          k.dma("sp", self.hal_src.ap(), src[T - 2:T, :], owner=self.b_d2d, reads=[bsrc[15]], writes=[self.b_halsrc])
          k.collective(self.hal_src.ap().opt(), self.hal_dst.ap().opt(), [list(range(NCORE))],
                       reads=[self.b_halsrc], writes=[self.b_haldst])
          htmp, b_htmp = self.big[:, 1, :], self.b_big[1]
          for r in range(NCORE):
              k.dma("sp", htmp[0:2, :], self.hal_dst[2 * r:2 * r + 2, :], owner=b_htmp, reads=[self.b_haldst], writes=[b_htmp])
              if r == 0:
                  self.ts(hh_tm[0:2, :], htmp[0:2, :], self.flag_sb[0:2, 0:1], None, ALU.mult, None,
                          reads=[b_htmp, self.b_flag], writes=[b_hh])
              else:
                  self.stt(hh_tm[0:2, :], htmp[0:2, :], self.flag_sb[0:2, r:r + 1], hh_tm[0:2, :], ALU.mult, ALU.add,
                           reads=[b_htmp, self.b_flag, b_hh], writes=[b_hh])
          self.tt(self.scr[0:2, :], hh_tm[0:2, :], hh_tm[0:2, :], ALU.mult, reads=[b_hh], writes=[self.b_scr])
          self.red(self.sm[0:2, 0:1], self.scr[0:2, :], reads=[self.b_scr], writes=[self.b_sm])
          self.rsqrt_ap(self.sm[0:2, 0:1], self.b_sm, 1.0 / D)
          self.ts(hh_tm[0:2, :], hh_tm[0:2, :], self.sm[0:2, 0:1], None, ALU.mult, None, reads=[b_hh, self.b_sm], writes=[b_hh])
          ps, bps = self.bank()
          for kk in range(16):
              self.tr(ps[:, kk * 2:kk * 2 + 2], bps, hh_tm[0:2, kk * 128:(kk + 1) * 128], self.ident[0:2, 0:2],
                      reads=[b_hh, self.b_ident], inc=(kk == 15))
          for kk in range(16):
              self.ts(hhT[:, kk, :], ps[:, kk * 2:kk * 2 + 2], self.preg[:, gi, kk:kk + 1], None, ALU.mult, None,
                      reads=[bps, self.b_preg], writes=[b_hhT])
        for tile in range(min(self.ntiles, T // 512)):
            chunks = [tile * NCH + i for i in range(NCH)]
            self.prenorm(src, bsrc, chunks, gi)
            it = 0
            for jg in range(11):
                slG, bG = self.slot()
                self.load_w(slG, bG, w_up, 0, 16, jg * 512, 512)
                slU, bU = self.slot()
                self.load_w(slU, bU, w_up, 0, 16, FF + jg * 512, 512)
                for j4 in range(4):
                    j = jg * 4 + j4
                    z, bz = zs[it % 2], b_zs[it % 2]
                    a, ba = acc[it % 2], b_acc[it % 2]
                    it += 1
                    for which, (sl, bsl) in enumerate(((slG, bG), (slU, bU))):
                        fc = which * 44 + j
                        ps, bps = self.bank()
                        for kk in range(16):
                            self.mm(ps[:], bps, sl[:, kk, j4 * 128:(j4 + 1) * 128], self.hnT[:, kk, :],
                                    start=(kk == 0), stop=(kk == 15), reads=[self.b_hnT, bsl])
                        self.act(z[:, which, 2:514], ps[:], AF.Copy, reads=[bps], writes=[bz])
                        if tile == 0 and not NOHALO:
                            ph, bph = self.bank()
                            for kk in range(16):
                                self.mm(ph[:, 0:2], bph, sl[:, kk, j4 * 128:(j4 + 1) * 128], hhT[:, kk, :],
                                        start=(kk == 0), stop=(kk == 15), reads=[b_hhT, bsl])
                            self.act(z[:, which, 0:2], ph[:, 0:2], AF.Copy, reads=[bph], writes=[bz])
                        else:
                            self.act(z[:, which, 0:2], zh[:, fc, :], AF.Copy, reads=[b_zh], writes=[bz])
                        self.act(zh[:, fc, :], z[:, which, 512:514], AF.Copy, reads=[bz], writes=[b_zh])
                        self.ts(a[:, which, :], z[:, which, 2:514], cw[:, 2, fc:fc + 1], cb[:, fc:fc + 1], ALU.mult, ALU.add,
                                reads=[bz, b_cw, b_cb], writes=[ba])
                        self.stt(a[:, which, :], z[:, which, 1:513], cw[:, 1, fc:fc + 1], a[:, which, :], ALU.mult, ALU.add,
                                 reads=[bz, b_cw, ba], writes=[ba])
                        self.stt(a[:, which, :], z[:, which, 0:512], cw[:, 0, fc:fc + 1], a[:, which, :], ALU.mult, ALU.add,
                                 reads=[bz, b_cw, ba], writes=[ba])
                    self.act(sg[:], a[:, 0, :], AF.Silu, reads=[ba], writes=[b_sg])
                    self.tt(actT[:, j, :], sg[:], a[:, 1, :], ALU.mult, reads=[b_sg, ba], writes=[b_actT])
            self.outproj(actT, b_actT, 44, w_dn, grow, src, bsrc, dst, bdst, chunks)
        k.barrier()

    def retention(self, src, bsrc, dst, bdst):
        k = self.k
        NCH = 4
        self.new_region(4)
        lg = [math.log1p(-2.0 ** (-5 - h)) for h in range(8)]
        import os
        RET_NOCC = os.environ.get("RET_NOCC") == "1"
        gT = self.alloc("rgT", [128, 32, 512], BF16)
        b_gT = Buf("rgT")
        S = self.alloc("S", [128, 2, 512], F32)
        b_S = Buf("S")
        Stmp = self.alloc("Stmp", [128, 2, 512], F32)
        b_Stmp = Buf("Stmp")
        Sb = self.alloc("Sb", [128, 2, 512], BF16)
        b_Sb = Buf("Sb")
        qks = self.alloc("qks", [128, 512], F32)
        b_qks = Buf("qks")
        t1 = self.alloc("t1", [128, 128], F32)
        t2 = self.alloc("t2", [128, 128], F32)
        b_t1, b_t2 = Buf("t1"), Buf("t2")
        rq = self.alloc("rq", [128, 512], F32)
        b_rq = Buf("rq")
        kdb = self.alloc("kdb", [128, 256], BF16)
        b_kdb = Buf("kdb")
        qkT = self.alloc("qkT", [128, 4, 128], BF16)
        b_qkT = Buf("qkT")
        vb = self.alloc("vb", [128, 512], BF16)
        b_vb = Buf("vb")
        sgt = self.alloc("sgt", [128, 512], F32)
        b_sgt = Buf("sgt")
        scm = self.alloc("scm", [128, 128], BF16)
        b_scm = Buf("scm")
        osb = self.alloc("osb", [128, 512], F32)
        b_osb = Buf("osb")
        gt = self.alloc("rgt", [128, 512], F32)
        b_gt = Buf("rgt")
        cosT = self.alloc("cosT", [128, 4, 128], F32)
        sinT = self.alloc("sinT", [128, 4, 128], F32)
        b_cs = Buf("cossin")
        ang = self.alloc("ang", [128, 128], F32)
        ang2 = self.alloc("ang2", [128, 128], F32)
        ang3 = self.alloc("ang3", [128, 128], F32)
        C1 = 6.28125
        C2 = 2 * math.pi - C1
        b_ang = Buf("ang")
        qdec = self.alloc("qdec", [128, 8], F32)
        kdec = self.alloc("kdec", [128, 8], F32)
        b_dec = Buf("dec")
        maskc = self.alloc("maskc", [128, 8, 128], F32)
        b_mask = Buf("maskc")
        invf = self.alloc("invf", [128, 128], F32)
        b_invf = Buf("invf")
        posf = self.alloc("posf", [128, 16], F32)
        b_posf = Buf("posf")
        ii = self.alloc("iota_i", [128, 128], I32)
        b_ii = Buf("iota_i")
        ff = self.alloc("iota_f", [128, 128], F32)
        b_ff = Buf("iota_f")
        k.emit("pool", lambda e: e.iota(ii[:, 0:1], pattern=[[0, 1]], base=1, channel_multiplier=1), writes=[b_ii])
        k.emit("pool", lambda e: e.iota(ii[:, 1:2], pattern=[[0, 1]], base=127, channel_multiplier=-1),
               reads=[b_ii], writes=[b_ii])
        k.emit("dve", lambda e: e.tensor_copy(out=ff[:, 0:2], in_=ii[:, 0:2]), reads=[b_ii], writes=[b_ff])
        for h in range(8):
            self.act(qdec[:, h:h + 1], ff[:, 0:1], AF.Exp, reads=[b_ff], writes=[b_dec], scale=lg[h])
            self.act(kdec[:, h:h + 1], ff[:, 1:2], AF.Exp, reads=[b_ff], writes=[b_dec], scale=lg[h])
        self.ts(kdec[:, :], kdec[:, :], 0.0625, None, ALU.mult, None, reads=[b_dec], writes=[b_dec])
        for h in range(8):
            k.emit("pool", lambda e, h=h: e.memset(maskc[:, h, :], math.exp(-128.0 * lg[h])), reads=[b_mask], writes=[b_mask])
            k.emit("pool", lambda e, h=h: e.affine_select(out=maskc[:, h, :], in_=maskc[:, h, :], pattern=[[1, 128]],
                                                          compare_op=ALU.is_ge, fill=0.0, base=0, channel_multiplier=-1),
                   reads=[b_mask], writes=[b_mask])
        k.emit("pool", lambda e: e.iota(ii[:, :], pattern=[[1, 128]], base=0, channel_multiplier=0),
               reads=[b_ii, b_ff], writes=[b_ii])
        k.emit("dve", lambda e: e.tensor_copy(out=ff[:, :], in_=ii[:, :]), reads=[b_ii], writes=[b_ff])
        self.act(invf[:, :], ff[:, :], AF.Exp, reads=[b_ff], writes=[b_invf], scale=-math.log(10000.0) / 127.0)
        k.dma("sp", ii[0:16, :], self.pos.ap(), owner=b_ii, reads=[b_ii], writes=[b_ii])
        k.emit("dve", lambda e: e.tensor_copy(out=self.xt[0:16, 0:128], in_=ii[0:16, :]), reads=[b_ii], writes=[self.b_xt])
        ps, bps = self.bank()
        self.tr(ps[:, 0:16], bps, self.xt[0:16, 0:128], self.ident[0:16, 0:16], reads=[self.b_xt, self.b_ident])
        k.emit("dve", lambda e, ps=ps: e.tensor_copy(out=posf[:, :], in_=ps[:, 0:16]), reads=[bps], writes=[b_posf])
        k.emit("pool", lambda e: e.memset(S[:], 0.0), writes=[b_S])
        for h in range(8):
            k.dma("sp", self.st[:, h * 1024:(h + 1) * 1024], S[:].rearrange("p a b -> p (a b)"), owner=b_S,
                  reads=[b_S], writes=[self.b_st[h]])

        for pas in (1, 2):
            for tile in range(min(self.ntiles, T // 512)):
                chunks = [tile * NCH + i for i in range(NCH)]
                self.prenorm(src, bsrc, chunks, 2)
                self._cosT, self._sinT = cosT, sinT
                if True:
                    for ci, c in enumerate(chunks):
                        self.ts(ang[:], invf[:], posf[:, c:c + 1], None, ALU.mult, None,
                                reads=[b_invf, b_posf], writes=[b_ang])
                        for which, dstT in ((0, sinT), (1, cosT)):
                            if which == 1:
                                self.ts(ang[:], ang[:], math.pi / 2, None, ALU.add, None, reads=[b_ang], writes=[b_ang])
                            self.ts(ang2[:], ang[:], 1.0 / (2 * math.pi), None, ALU.mult, None, reads=[b_ang], writes=[b_ang])
                            k.emit("dve", lambda e: e.tensor_copy(out=ii[:, :], in_=ang2[:]), reads=[b_ang], writes=[b_ii])
                            k.emit("dve", lambda e: e.tensor_copy(out=ang2[:], in_=ii[:, :]), reads=[b_ii], writes=[b_ang])
                            self.stt(ang3[:], ang2[:], -C1, ang[:], ALU.mult, ALU.add, reads=[b_ang], writes=[b_ang])
                            self.stt(ang3[:], ang2[:], -C2, ang3[:], ALU.mult, ALU.add, reads=[b_ang], writes=[b_ang])
                            self.ts(ang2[:], ang3[:], math.pi, None, ALU.is_gt, None, reads=[b_ang], writes=[b_ang])
                            self.stt(ang3[:], ang2[:], -2 * math.pi, ang3[:], ALU.mult, ALU.add, reads=[b_ang], writes=[b_ang])
                            self.ts(ang2[:], ang3[:], -math.pi, None, ALU.is_lt, None, reads=[b_ang], writes=[b_ang])
                            self.stt(ang3[:], ang2[:], 2 * math.pi, ang3[:], ALU.mult, ALU.add, reads=[b_ang], writes=[b_ang])
                            self.act(dstT[:, ci, :], ang3[:], AF.Sin, reads=[b_ang], writes=[b_cs])
                for h in range(8):
                    cdec = math.exp(128.0 * lg[h])
                    slQK, bQK = self.slot()
                    if pas == 2:
                        self.load_w(slQK, bQK, self.r_w_in, 0, 16, h * 256, 256, 0)
                    self.load_w(slQK, bQK, self.r_w_in, 0, 16, 2048 + h * 256, 256, 256)
                    slV, bV = self.slot()
                    self.load_w(slV, bV, self.r_w_in, 0, 16, 4096 + h * 512, 512)
                    if pas == 2:
                        slG, bGs = self.slot()
                        self.load_w(slG, bGs, self.r_w_in, 0, 16, 8192 + h * 512, 512)
                    Sflat = S[:].rearrange("p a b -> p (a b)")
                    if pas == 2 and tile == 0 and RET_NOCC:
                        k.emit("pool", lambda e: e.memset(S[:], 0.0), reads=[b_S], writes=[b_S])
                    elif pas == 2 and tile == 0:
                        Stf = Stmp[:].rearrange("p a b -> p (a b)")
                        for r in range(NCORE):
                            k.dma("sp", Stf, self.st2[r * 128:(r + 1) * 128, h * 1024:(h + 1) * 1024], owner=b_Stmp,
                                  reads=[self.b_st2], writes=[b_Stmp])
                            if r == 0:
                                self.ts(Sflat, Stf, self.flag_sb[:, 0:1], None, ALU.mult, None,
                                        reads=[b_Stmp, self.b_flag], writes=[b_S])
                            else:
                                self.stt(Sflat, Stf, self.flag_sb[:, r:r + 1], Sflat, ALU.mult, ALU.add,
                                         reads=[b_Stmp, self.b_flag, b_S], writes=[b_S])
                    else:
                        k.dma("sp", Sflat, self.st[:, h * 1024:(h + 1) * 1024], owner=b_S,
                              reads=[self.b_st[h]], writes=[b_S])
                    if pas == 2:
                        self.act(Sb[:], S[:], AF.Copy, reads=[b_S], writes=[b_Sb])
                    for ci in range(NCH):
                        c0 = 0 if pas == 2 else 256
                        ps, bps = self.bank()
                        for kk in range(16):
                            self.mm(ps[:, c0:512], bps, self.hnT[:, kk, ci * 128:(ci + 1) * 128], slQK[:, kk, c0:512],
                                    start=(kk == 0), stop=(kk == 15), reads=[self.b_hnT, bQK])
                        if pas == 2:
                            self.ts(qks[:, 0:256], ps[:, 0:256], qdec[:, h:h + 1], None, ALU.mult, None,
                                    reads=[bps, b_dec], writes=[b_qks])
                        self.ts(qks[:, 256:512], ps[:, 256:512], kdec[:, h:h + 1], None, ALU.mult, None,
                                reads=[bps, b_dec], writes=[b_qks])
                        if pas == 2:
                            cs, sn = cosT[:, ci, :], sinT[:, ci, :]
                        else:
                            cs = sn = None
                        if pas == 1:
                            pass
                        for part in ((0, 1) if pas == 2 else (1,)):
                            base = part * 256
                            xe = qks[:, base:base + 256:2]
                            xo = qks[:, base + 1:base + 256:2]
                            self.tt(t1[:], xe, self.cs_ap(ci, 0), ALU.mult, reads=[b_qks, b_cs], writes=[b_t1])
                            self.tt(t2[:], xo, self.cs_ap(ci, 1), ALU.mult, reads=[b_qks, b_cs], writes=[b_t2])
                            self.tt(rq[:, base:base + 128], t1[:], t2[:], ALU.subtract, reads=[b_t1, b_t2], writes=[b_rq])
                            self.tt(t1[:], xo, self.cs_ap(ci, 0), ALU.mult, reads=[b_qks, b_cs], writes=[b_t1])
                            self.tt(t2[:], xe, self.cs_ap(ci, 1), ALU.mult, reads=[b_qks, b_cs], writes=[b_t2])
                            self.tt(rq[:, base + 128:base + 256], t1[:], t2[:], ALU.add, reads=[b_t1, b_t2], writes=[b_rq])
                        self.act(kdb[:], rq[:, 256:512], AF.Copy, reads=[b_rq], writes=[b_kdb])
                        pv, bpv = self.bank()
                        for kk in range(16):
                            self.mm(pv[:], bpv, self.hnT[:, kk, ci * 128:(ci + 1) * 128], slV[:, kk, :],
                                    start=(kk == 0), stop=(kk == 15), reads=[self.b_hnT, bV])
                        self.act(vb[:], pv[:], AF.Copy, reads=[bpv], writes=[b_vb])
                        if pas == 2:
                            pg, bpg = self.bank()
                            for kk in range(16):
                                self.mm(pg[:], bpg, self.hnT[:, kk, ci * 128:(ci + 1) * 128], slG[:, kk, :],
                                        start=(kk == 0), stop=(kk == 15), reads=[self.b_hnT, bGs])
                            self.act(sgt[:], pg[:], AF.Silu, reads=[bpg], writes=[b_sgt])
                            pt, bpt = self.bank()
                            for j in range(4):
                                self.tr(pt[:, j * 128:(j + 1) * 128], bpt, rq[:, j * 128:(j + 1) * 128], self.ident[:],
                                        reads=[b_rq, self.b_ident], inc=(j == 3))
                            k.emit("dve", lambda e, pt=pt: e.tensor_copy(out=qkT[:].rearrange("p a b -> p (a b)"), in_=pt[:]),
                                   reads=[bpt], writes=[b_qkT])
                            psc, bpsc = self.bank()
                            for eo in range(2):
                                self.mm(psc[:, 0:128], bpsc, qkT[:, 2 + eo, :], qkT[:, eo, :], start=(eo == 0), stop=(eo == 1),
                                        reads=[b_qkT])
                            self.tt(scm[:], psc[:, 0:128], maskc[:, h, :], ALU.mult, reads=[bpsc, b_mask], writes=[b_scm])
                            po, bpo = self.bank()
                            self.mm(po[:], bpo, scm[:], vb[:], start=True, stop=False, reads=[b_scm, b_vb], inc=False)
                            self.mm(po[:], bpo, qkT[:, 0, :], Sb[:, 0, :], start=False, stop=False, reads=[b_qkT, b_Sb], inc=False)
                            self.mm(po[:], bpo, qkT[:, 1, :], Sb[:, 1, :], start=False, stop=True, reads=[b_qkT, b_Sb])
                        for eo in range(2):
                            pd, bpd = self.bank()
                            self.mm(pd[:], bpd, kdb[:, eo * 128:(eo + 1) * 128], vb[:], start=True, stop=True,
                                    reads=[b_kdb, b_vb])
                            self.stt(S[:, eo, :], S[:, eo, :], cdec, pd[:], ALU.mult, ALU.add,
                                     reads=[b_S, bpd], writes=[b_S])
                        if pas == 2:
                            self.act(Sb[:], S[:], AF.Copy, reads=[b_S], writes=[b_Sb])
                            self.act(osb[:], po[:], AF.Copy, reads=[bpo], writes=[b_osb])
                            self.tt(self.scr[:, 0:512], osb[:], osb[:], ALU.mult, reads=[b_osb], writes=[self.b_scr])
                            self.red(self.sm[:, 1:2], self.scr[:, 0:512], reads=[self.b_scr], writes=[self.b_sm])
                            self.rstd_from_ss(1, 512)
                            self.stt(gt[:], osb[:], self.sm[:, 1:2], sgt[:], ALU.mult, ALU.mult,
                                     reads=[b_osb, self.b_sm, b_sgt], writes=[b_gt])
                            pt, bpt = self.bank()
                            for j in range(4):
                                self.tr(pt[:, j * 128:(j + 1) * 128], bpt, gt[:, j * 128:(j + 1) * 128], self.ident[:],
                                        reads=[b_gt, self.b_ident], inc=(j == 3))
                            k.emit("act", lambda e, pt=pt, h=h, ci=ci: e.activation(
                                out=gT[:, h * 4:(h + 1) * 4, ci * 128:(ci + 1) * 128],
                                in_=pt[:].rearrange("p (j t) -> p j t", j=4), func=AF.Copy),
                                reads=[bpt], writes=[b_gT])
                    k.dma("sp", self.st[:, h * 1024:(h + 1) * 1024], Sflat, owner=b_S, reads=[b_S], writes=[self.b_st[h]])
                if pas == 2:
                    self.outproj(gT, b_gT, 32, self.r_w_out, 5, src, bsrc, dst, bdst, chunks)
            if pas == 1 and not RET_NOCC:
                k.collective(self.st.ap().opt(), self.st2.ap().opt(), [list(range(NCORE))],
                             reads=self.b_st, writes=[self.b_st2])
        k.barrier()

    def cs_ap(self, ci, which):
        return (self._cosT if which == 0 else self._sinT)[:, ci, :]

    def dump(self, src):
        if not self.debug:
            return
        i = len(self.dumps)
        d = self.nc.dram_tensor("dump%d" % i, [T, D], F32, kind="ExternalOutput")
        self.dumps.append(d)
        b = Buf("dump%d" % i)
        for c in range(16):
            self.k.dma("sp", d[c * 128:(c + 1) * 128, :], src[c * 128:(c + 1) * 128, :], owner=b,
                       reads=[self.b_xs[c]], writes=[b])
        self.k.barrier()

    def build(self, stages=("gmlp", "ffn0", "ret", "ffn1")):
        k = self.k
        self.setup_common()
        self.gather_weights()
        cur, bcur = self.x_in, None
        last = stages[-1]
        for s in stages:
            dst, bdst = (self.out, self.b_out) if s == last else (self.xs, self.b_xs)
            if s == "gmlp":
                self.gmlp(cur, bcur, dst, bdst)
            elif s == "ffn0":
                self.ffn(0, cur, bcur, dst, bdst, 1, 3)
            elif s == "ret":
                self.retention(cur, bcur, dst, bdst)
            elif s == "ffn1":
                self.ffn(1, cur, bcur, dst, bdst, 3, 7)
            if s != last:
                self.dump(dst)
            cur, bcur = dst, bdst
        k.barrier()
        k.finish()
        return self.nc


_CACHE = {}


def make_in_maps(inputs):
    x = np.ascontiguousarray(inputs["x"], dtype=np.float32).reshape(NCORE, T, D)
    pos = np.ascontiguousarray(inputs["positions"]).astype(np.int32).reshape(NCORE, 16, 128)
    f32 = lambda a: np.ascontiguousarray(a, dtype=np.float32)
    norm_g = np.stack([inputs["mix_pre_g"][0], inputs["mix_post_g"][0], inputs["ffn_pre_g"][0], inputs["ffn_post_g"][0],
                       inputs["mix_pre_g"][1], inputs["mix_post_g"][1], inputs["ffn_pre_g"][1], inputs["ffn_post_g"][1]])
    shared = {
        "norm_g": f32(norm_g),
        "g_ln": f32(np.stack([inputs["gmlp_ln_g"][0], inputs["gmlp_ln_b"][0]])),
        "g_ws": f32(np.asarray(inputs["gmlp_w_s"][0]).reshape(8 * 128, 128)),
        "g_bs": f32(inputs["gmlp_b_s"][0]),
    }
    big = {
        "g_w_in": inputs["gmlp_w_in"][0], "g_w_out": inputs["gmlp_w_out"][0],
        "r_w_in": inputs["ret_w_in"][0], "r_w_out": inputs["ret_w_out"][0],
    }
    for i in range(2):
        shared["f_cw%d" % i] = f32(np.asarray(inputs["ffn_conv_w"][i]).reshape(3 * 88, 128))
        shared["f_cb%d" % i] = f32(np.asarray(inputs["ffn_conv_b"][i]).reshape(88, 128))
        big["f_w_up%d" % i] = inputs["ffn_w_up"][i]
        big["f_w_dn%d" % i] = inputs["ffn_w_down"][i]
    maps = []
    for c in range(NCORE):
        m = dict(shared)
        m["x"] = x[c]
        m["pos"] = pos[c]
        sel = np.zeros((128, 8), np.float32)
        if c % 2 == 1:
            sel[:, c - 1] = 1.0
        m["sel"] = sel
        for name, w in big.items():
            if SHARD_W:
                r = w.shape[0] // NCORE
                m[name] = f32(w[c * r:(c + 1) * r])
            else:
                m[name] = f32(w)
        maps.append(m)
    return maps


def kernel(**inputs):
    inputs = {k_: np.asarray(v) for k_, v in inputs.items()}
    if "nc" not in _CACHE:
        _CACHE["nc"] = Prog().build()
    nc = _CACHE["nc"]
    res = run_bass_kernel_spmd(nc, make_in_maps(inputs), core_ids=list(range(NCORE)))
    out = np.concatenate([np.asarray(r["out"]) for r in res.results], axis=0)
    return out.reshape(4, 4096, D).astype(np.float32)
```

```python
import math
import numpy as np
import concourse.bass as bass
import concourse.mybir as mybir
from concourse.bass_utils import run_bass_kernel_spmd

F32, BF16, I32, U8 = mybir.dt.float32, mybir.dt.bfloat16, mybir.dt.int32, mybir.dt.uint8
AF = mybir.ActivationFunctionType
ALU = mybir.AluOpType
AX = mybir.AxisListType

D = 2048
T = 2048
NCORE = 8
EPS = 1e-6
GF = 6144
FF = 5632
KB = 1024
SHARD_W = True


class Buf:
    def __init__(self, name):
        self.name = name
        self.w = None
        self.r = {}
        self.dsem = None
        self.dcnt = 0


class K:
    ENG = ["pe", "act", "dve", "pool", "sp"]

    def __init__(self, nc):
        self.nc = nc
        self.prog = {e: [] for e in self.ENG}
        self.sem = {e: nc.alloc_semaphore(name="pg_" + e) for e in self.ENG}
        self.cnt = {e: 0 for e in self.ENG}
        self.waited = {e: {} for e in self.ENG}
        self.sempools = {}
        self.dbufs = []
        self.extra_toks = []

    def _wait(self, eng, s, v):
        if self.waited[eng].get(s, 0) >= v:
            return
        self.waited[eng][s] = v
        self.prog[eng].append(lambda e, s=s, v=v: e.wait_ge(s, v))

    def _deps(self, eng, reads, writes):
        deps = {}

        def add(tok):
            if tok is None:
                return
            s, v = tok
            if deps.get(s, 0) < v:
                deps[s] = v
        for b in reads:
            add(b.w)
        for b in writes:
            add(b.w)
            for s, v in b.r.items():
                add((s, v))
        own = self.sem[eng]
        for s, v in deps.items():
            if eng == "pe" and s is own:
                continue
            self._wait(eng, s, v)

    def _mark(self, tok, reads, writes):
        for b in reads:
            if b.r.get(tok[0], 0) < tok[1]:
                b.r[tok[0]] = tok[1]
        for b in writes:
            b.w = tok
            b.r = {}

    def emit(self, eng, fn, reads=(), writes=(), inc=True):
        self._deps(eng, reads, writes)
        s = self.sem[eng]
        tok = (s, self.cnt[eng] + 1)
        if inc:
            self.cnt[eng] += 1
            self.prog[eng].append(lambda e, fn=fn, s=s: fn(e).then_inc(s, 1))
        else:
            self.prog[eng].append(lambda e, fn=fn: fn(e))
        self._mark(tok, reads, writes)

    def _getsem(self, owner, kind):
        d = owner.__dict__.setdefault("dsems", {})
        if d.get(kind) is None:
            pool = self.sempools.setdefault(kind, [])
            if pool:
                sem, cnt = pool.pop()
            else:
                self.nsem_alloc = getattr(self, "nsem_alloc", 0) + 1
                sem, cnt = self.nc.alloc_semaphore(name="d%d_%s_%s" % (self.nsem_alloc, kind, owner.name)), 0
            d[kind] = [sem, cnt]
            self.dbufs.append((owner, kind))
        return d[kind]

    def dma(self, eng, out_ap, in_ap, owner, reads=(), writes=()):
        self._deps(eng, reads, writes)
        ent = self._getsem(owner, "sw" if eng == "pool" else "hw")
        ent[1] += 16
        tok = (ent[0], ent[1])
        self.prog[eng].append(
            lambda e, o=out_ap, i=in_ap, s=ent[0]: e.dma_start(out=o, in_=i).then_inc(s, 16))
        self._mark(tok, reads, writes)
        return tok

    def collective(self, src_ap, dst_ap, groups, reads, writes):
        eng = "pool"
        self._deps(eng, reads, writes)
        sem = self.nc.alloc_semaphore(name="cc%d" % len(self.extra_toks))
        tok = (sem, 1)
        self.extra_toks.append(tok)
        self.prog[eng].append(
            lambda e, a=src_ap, b=dst_ap, g=groups, s=sem: e.collective_compute(
                "AllGather", ALU.bypass, replica_groups=g, ins=[a], outs=[b]).then_inc(s, 1))
        self._mark(tok, reads, writes)
        return tok

    def barrier(self, skip_extra=False):
        toks = [(self.sem[o], self.cnt[o]) for o in self.ENG if self.cnt[o] > 0]
        toks += [tuple(o.dsems[kd]) for o, kd in self.dbufs if o.dsems[kd][1] > 0]
        if not skip_extra:
            toks += self.extra_toks
        for e in self.ENG:
            for s, v in toks:
                if s is self.sem[e]:
                    continue
                self._wait(e, s, v)
        for o, kd in self.dbufs:
            self.sempools[kd].append(tuple(o.dsems[kd]))
            o.dsems[kd] = None
        self.dbufs = []

    def finish(self):
        nc = self.nc
        with nc.Block() as block:
            @block.tensor
            def _(e):
                for f in self.prog["pe"]:
                    f(e)

            @block.scalar
            def _(e):
                for f in self.prog["act"]:
                    f(e)

            @block.vector
            def _(e):
                for f in self.prog["dve"]:
                    f(e)

            @block.gpsimd
            def _(e):
                for f in self.prog["pool"]:
                    f(e)

            @block.sync
            def _(e):
                for f in self.prog["sp"]:
                    f(e)


class Prog:
    def __init__(self, debug=False, ntiles=99):
        self.debug = debug
        self.ntiles = ntiles
        nc = self.nc = bass.Bass("TRN2", target_bir_lowering=False)
        self.k = K(nc)
        dt = nc.dram_tensor
        self.x_in = dt("x", [T, D], F32, kind="ExternalInput")
        self.pos = dt("pos", [16, 128], I32, kind="ExternalInput")
        self.flag = dt("sel", [128, 8], F32, kind="ExternalInput")
        self.norm_g = dt("norm_g", [8, D], F32, kind="ExternalInput")
        self.g_ln = dt("g_ln", [2, GF], F32, kind="ExternalInput")
        self.g_ws = dt("g_ws", [8 * 128, 128], F32, kind="ExternalInput")
        self.g_bs = dt("g_bs", [8, 128], F32, kind="ExternalInput")
        self.f_cw = [dt("f_cw%d" % i, [3 * 88, 128], F32, kind="ExternalInput") for i in range(2)]
        self.f_cb = [dt("f_cb%d" % i, [88, 128], F32, kind="ExternalInput") for i in range(2)]
        self.wspecs = [("g_w_in", D, 2 * GF), ("g_w_out", GF, D), ("f_w_up0", D, 2 * FF), ("f_w_dn0", FF, D),
                       ("r_w_in", D, 12288), ("r_w_out", 4096, D), ("f_w_up1", D, 2 * FF), ("f_w_dn1", FF, D)]
        self.W = {}
        self.Wb = {}
        self.Wsh = {}
        self.Wbn = {}
        self.Wbnf = {}
        self.Wf = {}
        for name, R, C in self.wspecs:
            if SHARD_W:
                self.Wsh[name] = dt(name, [R // NCORE, C], F32, kind="ExternalInput")
                self.Wbnf[name] = dt(name + "_bn", [R // NCORE, C // 2], F32)
                self.Wf[name] = dt(name + "_full", [R, C // 2], F32)
                self.Wbn[name] = self.Wbnf[name].bitcast(BF16)
                self.W[name] = self.Wf[name].bitcast(BF16)
            else:
                self.W[name] = dt(name, [R, C], F32, kind="ExternalInput")
            self.Wb[name] = Buf("w_" + name)
        self.g_w_in, self.g_w_out = self.W["g_w_in"], self.W["g_w_out"]
        self.r_w_in, self.r_w_out = self.W["r_w_in"], self.W["r_w_out"]
        self.f_w_up = [self.W["f_w_up0"], self.W["f_w_up1"]]
        self.f_w_dn = [self.W["f_w_dn0"], self.W["f_w_dn1"]]
        self.out = dt("out", [T, D], F32, kind="ExternalOutput")
        self.xs = dt("xs_scr", [T, D], F32)
        self.hal_src = dt("hal_src", [2, D], F32)
        self.hal_dst = dt("hal_dst", [16, D], F32)
        self.st = dt("st_scr", [128, 8192], F32)
        self.st2 = dt("st2_scr", [1024, 8192], F32)
        self.dumps = []
        self.b_xs = [Buf("xs%d" % c) for c in range(16)]
        self.b_out = [Buf("out%d" % c) for c in range(16)]
        self.b_halsrc, self.b_haldst = Buf("halsrc"), Buf("haldst")
        self.b_st = [Buf("st%d" % h) for h in range(8)]
        self.b_st2 = Buf("st2")
        self.b_d2d = Buf("d2d")
        total = 206 * KB
        nc.alloc_sbuf_tensor("arena", [128, total], U8)
        self.abase = nc.sbuf_base - total
        self.atotal = total
        self.cur = 0
        self.ps = [nc.alloc_psum_tensor("ps%d" % i, [128, 512], F32) for i in range(8)]
        self.b_ps = [Buf("ps%d" % i) for i in range(8)]
        self.psi = 0
        self.nalloc = 0

    def alloc(self, name, shape, dtype):
        nbytes = int(np.prod(shape[1:])) * (2 if dtype == BF16 else 4)
        nbytes = (nbytes + 63) // 64 * 64
        assert self.cur + nbytes <= self.atotal, (name, self.cur, nbytes)
        self.nalloc += 1
        t = self.nc.alloc_sbuf_tensor_at("%s_%d" % (name, self.nalloc), list(shape), dtype,
                                         offset=self.abase + self.cur)
        self.cur += nbytes
        return t

    def bank(self):
        i = self.psi
        self.psi = (self.psi + 1) % 8
        return self.ps[i], self.b_ps[i]

    def mm(self, ps, bps, lhsT, rhs, start, stop, reads, inc=None):
        self.k.emit("pe", lambda e: e.matmul(ps, lhsT, rhs, start=start, stop=stop),
                    reads=reads, writes=[bps], inc=(stop if inc is None else inc))

    def tr(self, ps, bps, in_, ident, reads, inc=True):
        self.k.emit("pe", lambda e: e.transpose(ps, in_, ident), reads=reads, writes=[bps], inc=inc)

    def act(self, out, in_, func, reads, writes, scale=None):
        if scale is None:
            self.k.emit("act", lambda e: e.activation(out=out, in_=in_, func=func), reads=reads, writes=writes)
        else:
            self.k.emit("act", lambda e: e.activation(out=out, in_=in_, func=func, scale=scale),
                        reads=reads, writes=writes)

    def ts(self, out, in0, s1, s2, op0, op1, reads, writes, eng="dve"):
        if op1 is None:
            self.k.emit(eng, lambda e: e.tensor_scalar(out=out, in0=in0, scalar1=s1, scalar2=None, op0=op0),
                        reads=reads, writes=writes)
        else:
            self.k.emit(eng, lambda e: e.tensor_scalar(out=out, in0=in0, scalar1=s1, scalar2=s2, op0=op0, op1=op1),
                        reads=reads, writes=writes)

    def tt(self, out, in0, in1, op, reads, writes, eng="dve"):
        self.k.emit(eng, lambda e: e.tensor_tensor(out=out, in0=in0, in1=in1, op=op), reads=reads, writes=writes)

    def stt(self, out, in0, scalar, in1, op0, op1, reads, writes):
        self.k.emit("dve", lambda e: e.scalar_tensor_tensor(out=out, in0=in0, scalar=scalar, in1=in1, op0=op0, op1=op1),
                    reads=reads, writes=writes)

    def red(self, out, in_, reads, writes):
        self.k.emit("dve", lambda e: e.tensor_reduce(out=out, in_=in_, axis=AX.X, op=ALU.add),
                    reads=reads, writes=writes)

    def bcast(self, handle, off, n, parts=128):
        return bass.AP(handle, off, [[0, parts], [1, n]])

    def setup_common(self):
        k = self.k
        self.ident = self.alloc("ident", [128, 128], F32)
        self.b_ident = Buf("ident")
        self.preg = self.alloc("preg", [128, 4, 16], F32)
        self.b_preg = Buf("preg")
        self.flag_sb = self.alloc("flag", [128, 8], F32)
        self.b_flag = Buf("flagsb")
        self.hnT = self.alloc("hnT", [128, 16, 512], BF16)
        self.b_hnT = Buf("hnT")
        self.big = self.alloc("big32", [128, 4, 2048], F32)
        self.b_big = [Buf("big%d" % i) for i in range(4)]
        self.xt = self.alloc("xt", [128, 2048], F32)
        self.b_xt = Buf("xt")
        self.scr = self.alloc("scr", [128, 2048], BF16)
        self.b_scr = Buf("scr")
        self.gb = self.alloc("gb", [128, 2048], F32)
        self.b_gb = Buf("gb")
        self.sm = self.alloc("small", [128, 16], F32)
        self.b_sm = Buf("small")
        self.region0 = self.cur
        k.emit("pool", lambda e: e.memset(self.ident[:], 1.0), writes=[self.b_ident])
        k.emit("pool", lambda e: e.affine_select(out=self.ident[:], in_=self.ident[:], pattern=[[-1, 128]],
                                                 compare_op=ALU.is_equal, fill=0.0, base=0, channel_multiplier=1),
               reads=[self.b_ident], writes=[self.b_ident])
        k.dma("sp", self.flag_sb[:], self.flag.ap(), owner=self.b_flag, writes=[self.b_flag])
        stage = self.xt
        for i, row in enumerate((0, 2, 4, 6)):
            src = bass.AP(self.norm_g, row * D, [[128, 16], [1, 128]])
            k.dma("sp", stage[0:16, 0:128], src, owner=self.b_xt, writes=[self.b_xt])
            ps, bps = self.bank()
            self.tr(ps[:, 0:16], bps, stage[0:16, 0:128], self.ident[0:16, 0:16], reads=[self.b_xt, self.b_ident])
            k.emit("dve", lambda e, ps=ps, i=i: e.tensor_copy(out=self.preg[:, i, :], in_=ps[:, 0:16]),
                   reads=[bps], writes=[self.b_preg])

    def gather_weights(self):
        if not SHARD_W:
            return
        k = self.k
        self.new_region(4)
        for name, R, C in self.wspecs:
            rows = R // NCORE
            kk = rows // 64
            bb = Buf("bn_" + name)
            for c0 in range(0, C, 512):
                sl, bsl = self.slot()
                src = self.Wsh[name][0:rows, c0:c0 + 512].rearrange("(k p) n -> p k n", p=64)
                dstd = self.Wbn[name][0:rows, c0:c0 + 512].rearrange("(k p) n -> p k n", p=64)
                k.dma("pool", sl[0:64, 0:kk, :], src, owner=bsl, writes=[bsl])
                k.dma("sp", dstd, sl[0:64, 0:kk, :], owner=bsl, reads=[bsl], writes=[bb])
            k.collective(self.Wbnf[name].ap().opt(), self.Wf[name].ap().opt(), [list(range(NCORE))],
                         reads=[bb], writes=[self.Wb[name]])
        k.barrier(skip_extra=True)

    def load_fm(self, dst_ap, bdst, src_rows_ap, R):
        k = self.k
        k.dma("sp", self.xt[0:R, 0:128], src_rows_ap, owner=self.b_xt, writes=[self.b_xt])
        ps, bps = self.bank()
        self.tr(ps[:, 0:R], bps, self.xt[0:R, 0:128], self.ident[0:R, 0:R], reads=[self.b_xt, self.b_ident])
        k.emit("dve", lambda e: e.tensor_copy(out=dst_ap, in_=ps[:, 0:R]), reads=[bps], writes=[bdst])

    def new_region(self, nslots):
        self.cur = self.region0
        self.slots = [self.alloc("slot", [128, 16, 512], BF16) for _ in range(nslots)]
        self.b_slots = [Buf("slot%d" % i) for i in range(nslots)]
        self.si = 0

    def slot(self):
        i = self.si
        self.si = (self.si + 1) % len(self.slots)
        return self.slots[i], self.b_slots[i]

    def load_w(self, sl, bsl, w, r0, nk, c0, ncols, coff=0):
        src = w[r0:r0 + nk * 128, c0:c0 + ncols].rearrange("(k p) n -> p k n", p=128)
        wb = [b for n_, b in self.Wb.items() if self.W[n_] is w]
        self.k.dma("sp" if SHARD_W else "pool", sl[:, 0:nk, coff:coff + ncols], src, owner=bsl, reads=wb, writes=[bsl])

    def rsqrt_ap(self, a, buf, scale, np_=128):
        self.ts(a, a, scale, EPS, ALU.mult, ALU.add, reads=[buf], writes=[buf])
        self.act(a, a, AF.Sqrt, reads=[buf], writes=[buf])
        self.k.emit("dve", lambda e: e.reciprocal(out=a, in_=a), reads=[buf], writes=[buf])

    def rstd_from_ss(self, col, n):
        self.rsqrt_ap(self.sm[:, col:col + 1], self.b_sm, 1.0 / n)

    def prenorm(self, src, bsrc, chunks, gi):
        k = self.k
        n = len(chunks)
        for ci, c in enumerate(chunks):
            k.dma("sp", self.xt[:], src[c * 128:(c + 1) * 128, :], owner=self.b_xt,
                  reads=([bsrc[c]] if bsrc else []), writes=[self.b_xt])
            self.tt(self.scr[:], self.xt[:], self.xt[:], ALU.mult, reads=[self.b_xt], writes=[self.b_scr])
            self.red(self.sm[:, 0:1], self.scr[:], reads=[self.b_scr], writes=[self.b_sm])
            self.rstd_from_ss(0, D)
            self.ts(self.big[:, ci, :], self.xt[:], self.sm[:, 0:1], None, ALU.mult, None,
                    reads=[self.b_xt, self.b_sm], writes=[self.b_big[ci]])
        for kk in range(16):
            ps, bps = self.bank()
            for ci in range(n):
                self.tr(ps[:, ci * 128:(ci + 1) * 128], bps, self.big[:, ci, kk * 128:(kk + 1) * 128], self.ident[:],
                        reads=[self.b_big[ci], self.b_ident], inc=(ci == n - 1))
            self.ts(self.hnT[:, kk, 0:n * 128], ps[:, 0:n * 128], self.preg[:, gi, kk:kk + 1], None, ALU.mult, None,
                    reads=[bps, self.b_preg], writes=[self.b_hnT])

    def outproj(self, actT, bact, KC, w_out, grow, src, bsrc, dst, bdst, chunks):
        k = self.k
        n = len(chunks)
        bacts = list(bact) if isinstance(bact, (list, tuple)) else [bact]
        k.dma("sp", self.gb[:], self.bcast(self.norm_g, grow * D, D), owner=self.b_gb, writes=[self.b_gb])
        for nb in range(4):
            banks = [self.bank() for _ in range(n)]
            for kg in range(0, KC, 16):
                kn = min(16, KC - kg)
                sl, bsl = self.slot()
                self.load_w(sl, bsl, w_out, kg * 128, kn, nb * 512, 512)
                for ci in range(n):
                    ps, bps = banks[ci]
                    for kk in range(kn):
                        self.mm(ps[:], bps, actT[:, kg + kk, ci * 128:(ci + 1) * 128], sl[:, kk, :],
                                start=(kg + kk == 0), stop=(kg + kk == KC - 1), reads=bacts + [bsl],
                                inc=(kk == kn - 1))
            for ci in range(n):
                ps, bps = banks[ci]
                self.act(self.big[:, ci, nb * 512:(nb + 1) * 512], ps[:], AF.Copy, reads=[bps], writes=[self.b_big[ci]])
        for ci, c in enumerate(chunks):
            y = self.big[:, ci, :]
            self.tt(self.scr[:], y, y, ALU.mult, reads=[self.b_big[ci]], writes=[self.b_scr])
            self.red(self.sm[:, 0:1], self.scr[:], reads=[self.b_scr], writes=[self.b_sm])
            self.rstd_from_ss(0, D)
            self.stt(y, y, self.sm[:, 0:1], self.gb[:], ALU.mult, ALU.mult,
                     reads=[self.b_big[ci], self.b_sm, self.b_gb], writes=[self.b_big[ci]])
            k.dma("sp", self.xt[:], src[c * 128:(c + 1) * 128, :], owner=self.b_xt,
                  reads=([bsrc[c]] if bsrc else []), writes=[self.b_xt])
            self.tt(self.xt[:], self.xt[:], y, ALU.add, reads=[self.b_xt, self.b_big[ci]], writes=[self.b_xt])
            k.dma("sp", dst[c * 128:(c + 1) * 128, :], self.xt[:], owner=self.b_xt,
                  reads=[self.b_xt], writes=[bdst[c]])

    def gmlp(self, src, bsrc, dst, bdst):
        k = self.k
        NCH = 4
        self.new_region(4)
        off = self.cur
        vg = self.alloc("vg", [128, 12, NCH * 512], BF16)
        gT = self.nc.alloc_sbuf_tensor_at("gTalias", [128, 48, 512], BF16, offset=self.abase + off)
        b_vg = [Buf("vg%d" % i) for i in range(12)]
        lnp = self.alloc("lnp", [128, 2, 512], F32)
        b_lnp = Buf("lnp")
        tmp = self.alloc("tmp", [128, 512], F32)
        b_tmp = Buf("tmp")
        ub = self.alloc("ub", [128, 512], F32)
        b_ub = Buf("ub")
        gt = self.alloc("gt", [128, NCH, 512], F32)
        b_gt = [Buf("gt%d" % i) for i in range(NCH)]
        st = self.alloc("bnst", [128, 12, 6], F32)
        b_st = Buf("bnst")
        mv = self.alloc("mv", [128, NCH, 2], F32)
        b_mv = Buf("mv")
        wsT = self.alloc("wsT", [128, 8, 128], BF16)
        b_wsT = Buf("wsT")
        wtmp = self.alloc("wtmp", [128, 128], F32)
        b_wtmp = Buf("wtmp")
        bs_tm = self.alloc("bs_tm", [128, 8], F32)
        b_bs = Buf("bs_tm")

        def vblk(nb, ci, c0=0, n=512):
            return vg[:, nb, ci * 512 + c0:ci * 512 + c0 + n]
        for g in range(8):
            k.dma("sp", self.xt[:, 0:128], self.g_ws[g * 128:(g + 1) * 128, :], owner=self.b_xt, writes=[self.b_xt])
            ps, bps = self.bank()
            self.tr(ps[:, 0:128], bps, self.xt[:, 0:128], self.ident[:], reads=[self.b_xt, self.b_ident])
            k.emit("act", lambda e, ps=ps: e.activation(out=wtmp[:], in_=ps[:, 0:128], func=AF.Copy),
                   reads=[bps], writes=[b_wtmp])
            k.emit("pool", lambda e: e.affine_select(out=wtmp[:], in_=wtmp[:], pattern=[[1, 128]],
                                                     compare_op=ALU.is_ge, fill=0.0, base=0, channel_multiplier=-1),
                   reads=[b_wtmp], writes=[b_wtmp])
            k.emit("dve", lambda e, g=g: e.tensor_copy(out=wsT[:, g, :], in_=wtmp[:]), reads=[b_wtmp], writes=[b_wsT])
        self.load_fm(bs_tm[:, :], b_bs, self.g_bs[0:8, :], 8)
        for tile in range(min(self.ntiles, T // (NCH * 128))):
            chunks = [tile * NCH + i for i in range(NCH)]
            self.prenorm(src, bsrc, chunks, 0)
            for nb in range(12):
                sl, bsl = self.slot()
                self.load_w(sl, bsl, self.g_w_in, 0, 16, GF + nb * 512, 512)
                for ci in range(NCH):
                    ps, bps = self.bank()
                    for kk in range(16):
                        self.mm(ps[:], bps, self.hnT[:, kk, ci * 128:(ci + 1) * 128], sl[:, kk, :],
                                start=(kk == 0), stop=(kk == 15), reads=[self.b_hnT, bsl])
                    self.act(vblk(nb, ci), ps[:], AF.Gelu, reads=[bps], writes=[b_vg[nb]])
            for ci in range(NCH):
                for nb in range(12):
                    k.emit("dve", lambda e, ci=ci, nb=nb: e.bn_stats(out=st[:, nb, :], in_=vblk(nb, ci)),
                           reads=[b_vg[nb]], writes=[b_st])
                k.emit("dve", lambda e, ci=ci: e.bn_aggr(out=mv[:, ci, :], in_=st[:].rearrange("p a b -> p (a b)")),
                       reads=[b_st], writes=[b_mv])
                self.rsqrt_ap(mv[:, ci, 1:2], b_mv, 1.0)
            for nb in range(12):
                k.dma("sp", lnp[:, 0, :], self.bcast(self.g_ln, nb * 512, 512), owner=b_lnp, writes=[b_lnp])
                k.dma("sp", lnp[:, 1, :], self.bcast(self.g_ln, GF + nb * 512, 512), owner=b_lnp, writes=[b_lnp])
                for ci in range(NCH):
                    vb = vblk(nb, ci)
                    self.ts(tmp[:], vb, mv[:, ci, 0:1], mv[:, ci, 1:2], ALU.subtract, ALU.mult,
                            reads=[b_vg[nb], b_mv], writes=[b_tmp])
                    self.tt(tmp[:], tmp[:], lnp[:, 0, :], ALU.mult, reads=[b_tmp, b_lnp], writes=[b_tmp])
                    self.tt(vb, tmp[:], lnp[:, 1, :], ALU.add, reads=[b_tmp, b_lnp], writes=[b_vg[nb]])
            for nb in range(12):
                sl, bsl = self.slot()
                self.load_w(sl, bsl, self.g_w_in, 0, 16, nb * 512, 512)
                for ci in range(NCH):
                    ps, bps = self.bank()
                    for kk in range(16):
                        self.mm(ps[:], bps, self.hnT[:, kk, ci * 128:(ci + 1) * 128], sl[:, kk, :],
                                start=(kk == 0), stop=(kk == 15), reads=[self.b_hnT, bsl])
                    self.act(ub[:], ps[:], AF.Gelu, reads=[bps], writes=[b_ub])
                    pm, bpm = self.bank()
                    for hh in range(2):
                        g = (nb * 512 + hh * 256) // 768
                        self.mm(pm[:, hh * 256:(hh + 1) * 256], bpm, wsT[:, g, :], vblk(nb, ci, hh * 256, 256),
                                start=True, stop=True, reads=[b_wsT, b_vg[nb]], inc=(hh == 1))
                    for hh in range(2):
                        g = (nb * 512 + hh * 256) // 768
                        self.stt(gt[:, ci, hh * 256:(hh + 1) * 256], pm[:, hh * 256:(hh + 1) * 256], bs_tm[:, g:g + 1],
                                 ub[:, hh * 256:(hh + 1) * 256], ALU.add, ALU.mult,
                                 reads=[bpm, b_bs, b_ub], writes=[b_gt[ci]])
                for ci in range(NCH):
                    pt, bpt = self.bank()
                    for j in range(4):
                        self.tr(pt[:, j * 128:(j + 1) * 128], bpt, gt[:, ci, j * 128:(j + 1) * 128], self.ident[:],
                                reads=[b_gt[ci], self.b_ident], inc=(j == 3))
                    k.emit("act", lambda e, pt=pt, nb=nb, ci=ci: e.activation(
                        out=gT[:, nb * 4:(nb + 1) * 4, ci * 128:(ci + 1) * 128],
                        in_=pt[:].rearrange("p (j t) -> p j t", j=4), func=AF.Copy),
                        reads=[bpt], writes=[b_vg[nb]])
            self.outproj(gT, b_vg, 48, self.g_w_out, 1, src, bsrc, dst, bdst, chunks)
        k.barrier()

    def ffn(self, layer, src, bsrc, dst, bdst, gi, grow):
        k = self.k
        NCH = 4
        self.new_region(4)
        w_up, w_dn = self.f_w_up[layer], self.f_w_dn[layer]
        actT = self.alloc("actT", [128, 44, 512], BF16)
        b_actT = Buf("actT")
        zs = [self.alloc("zs", [128, 2, 514], F32) for _ in range(2)]
        b_zs = [Buf("zs0"), Buf("zs1")]
        acc = [self.alloc("acc", [128, 2, 512], F32) for _ in range(2)]
        b_acc = [Buf("acc0"), Buf("acc1")]
        sg = self.alloc("sg", [128, 512], F32)
        b_sg = Buf("sg")
        zh = self.alloc("zhalo", [128, 88, 2], F32)
        b_zh = Buf("zhalo")
        cw = self.alloc("cw", [128, 3, 88], F32)
        b_cw = Buf("cw")
        cb = self.alloc("cb", [128, 88], F32)
        b_cb = Buf("cb")
        hh_tm = self.big[:, 0, :]
        b_hh = self.b_big[0]
        hhT = self.alloc("hal_T", [128, 16, 2], BF16)
        b_hhT = Buf("hal_T")
        for tap in range(3):
            self.load_fm(cw[:, tap, :], b_cw, self.f_cw[layer][tap * 88:(tap + 1) * 88, :], 88)
        self.load_fm(cb[:, :], b_cb, self.f_cb[layer][0:88, :], 88)
        import os
        NOHALO = os.environ.get("FFN_NOHALO") == "1"
        k.emit("pool", lambda e: e.memset(zh[:], 0.0), writes=[b_zh])
        if not NOHALO:
          k.dma("sp", self.hal_src.ap(), src[T - 2:T, :], owner=self.b_d2d, reads=[bsrc[15]], writes=[self.b_halsrc])
          k.collective(self.hal_src.ap().opt(), self.hal_dst.ap().opt(), [list(range(NCORE))],
                       reads=[self.b_halsrc], writes=[self.b_haldst])
          htmp, b_htmp = self.big[:, 1, :], self.b_big[1]
          for r in range(NCORE):
              k.dma("sp", htmp[0:2, :], self.hal_dst[2 * r:2 * r + 2, :], owner=b_htmp, reads=[self.b_haldst], writes=[b_htmp])
              if r == 0:
                  self.ts(hh_tm[0:2, :], htmp[0:2, :], self.flag_sb[0:2, 0:1], None, ALU.mult, None,
                          reads=[b_htmp, self.b_flag], writes=[b_hh])
              else:
                  self.stt(hh_tm[0:2, :], htmp[0:2, :], self.flag_sb[0:2, r:r + 1], hh_tm[0:2, :], ALU.mult, ALU.add,
                           reads=[b_htmp, self.b_flag, b_hh], writes=[b_hh])
          self.tt(self.scr[0:2, :], hh_tm[0:2, :], hh_tm[0:2, :], ALU.mult, reads=[b_hh], writes=[self.b_scr])
          self.red(self.sm[0:2, 0:1], self.scr[0:2, :], reads=[self.b_scr], writes=[self.b_sm])
          self.rsqrt_ap(self.sm[0:2, 0:1], self.b_sm, 1.0 / D)
          self.ts(hh_tm[0:2, :], hh_tm[0:2, :], self.sm[0:2, 0:1], None, ALU.mult, None, reads=[b_hh, self.b_sm], writes=[b_hh])
          ps, bps = self.bank()
          for kk in range(16):
              self.tr(ps[:, kk * 2:kk * 2 + 2], bps, hh_tm[0:2, kk * 128:(kk + 1) * 128], self.ident[0:2, 0:2],
                      reads=[b_hh, self.b_ident], inc=(kk == 15))
          for kk in range(16):
              self.ts(hhT[:, kk, :], ps[:, kk * 2:kk * 2 + 2], self.preg[:, gi, kk:kk + 1], None, ALU.mult, None,
                      reads=[bps, self.b_preg], writes=[b_hhT])
        for tile in range(min(self.ntiles, T // 512)):
            chunks = [tile * NCH + i for i in range(NCH)]
            self.prenorm(src, bsrc, chunks, gi)
            it = 0
            for jg in range(11):
                slG, bG = self.slot()
                self.load_w(slG, bG, w_up, 0, 16, jg * 512, 512)
                slU, bU = self.slot()
                self.load_w(slU, bU, w_up, 0, 16, FF + jg * 512, 512)
                for j4 in range(4):
                    j = jg * 4 + j4
                    z, bz = zs[it % 2], b_zs[it % 2]
                    a, ba = acc[it % 2], b_acc[it % 2]
                    it += 1
                    for which, (sl, bsl) in enumerate(((slG, bG), (slU, bU))):
                        fc = which * 44 + j
                        ps, bps = self.bank()
                        for kk in range(16):
                            self.mm(ps[:], bps, sl[:, kk, j4 * 128:(j4 + 1) * 128], self.hnT[:, kk, :],
                                    start=(kk == 0), stop=(kk == 15), reads=[self.b_hnT, bsl])
                        self.act(z[:, which, 2:514], ps[:], AF.Copy, reads=[bps], writes=[bz])
                        if tile == 0 and not NOHALO:
                            ph, bph = self.bank()
                            for kk in range(16):
                                self.mm(ph[:, 0:2], bph, sl[:, kk, j4 * 128:(j4 + 1) * 128], hhT[:, kk, :],
                                        start=(kk == 0), stop=(kk == 15), reads=[b_hhT, bsl])
                            self.act(z[:, which, 0:2], ph[:, 0:2], AF.Copy, reads=[bph], writes=[bz])
                        else:
                            self.act(z[:, which, 0:2], zh[:, fc, :], AF.Copy, reads=[b_zh], writes=[bz])
                        self.act(zh[:, fc, :], z[:, which, 512:514], AF.Copy, reads=[bz], writes=[b_zh])
                        self.ts(a[:, which, :], z[:, which, 2:514], cw[:, 2, fc:fc + 1], cb[:, fc:fc + 1], ALU.mult, ALU.add,
                                reads=[bz, b_cw, b_cb], writes=[ba])
                        self.stt(a[:, which, :], z[:, which, 1:513], cw[:, 1, fc:fc + 1], a[:, which, :], ALU.mult, ALU.add,
                                 reads=[bz, b_cw, ba], writes=[ba])
                        self.stt(a[:, which, :], z[:, which, 0:512], cw[:, 0, fc:fc + 1], a[:, which, :], ALU.mult, ALU.add,
                                 reads=[bz, b_cw, ba], writes=[ba])
                    self.act(sg[:], a[:, 0, :], AF.Silu, reads=[ba], writes=[b_sg])
                    self.tt(actT[:, j, :], sg[:], a[:, 1, :], ALU.mult, reads=[b_sg, ba], writes=[b_actT])
            self.outproj(actT, b_actT, 44, w_dn, grow, src, bsrc, dst, bdst, chunks)
        k.barrier()

    def retention(self, src, bsrc, dst, bdst):
        k = self.k
        NCH = 4
        self.new_region(4)
        lg = [math.log1p(-2.0 ** (-5 - h)) for h in range(8)]
        import os
        RET_NOCC = os.environ.get("RET_NOCC") == "1"
        gT = self.alloc("rgT", [128, 32, 512], BF16)
        b_gT = Buf("rgT")
        S = self.alloc("S", [128, 2, 512], F32)
        b_S = Buf("S")
        Stmp = self.alloc("Stmp", [128, 2, 512], F32)
        b_Stmp = Buf("Stmp")
        Sb = self.alloc("Sb", [128, 2, 512], BF16)
        b_Sb = Buf("Sb")
        qks = self.alloc("qks", [128, 512], F32)
        b_qks = Buf("qks")
        t1 = self.alloc("t1", [128, 128], F32)
        t2 = self.alloc("t2", [128, 128], F32)
        b_t1, b_t2 = Buf("t1"), Buf("t2")
        rq = self.alloc("rq", [128, 512], F32)
        b_rq = Buf("rq")
        kdb = self.alloc("kdb", [128, 256], BF16)
        b_kdb = Buf("kdb")
        qkT = self.alloc("qkT", [128, 4, 128], BF16)
        b_qkT = Buf("qkT")
        vb = self.alloc("vb", [128, 512], BF16)
        b_vb = Buf("vb")
        sgt = self.alloc("sgt", [128, 512], F32)
        b_sgt = Buf("sgt")
        scm = self.alloc("scm", [128, 128], BF16)
        b_scm = Buf("scm")
        osb = self.alloc("osb", [128, 512], F32)
        b_osb = Buf("osb")
        gt = self.alloc("rgt", [128, 512], F32)
        b_gt = Buf("rgt")
        cosT = self.alloc("cosT", [128, 4, 128], F32)
        sinT = self.alloc("sinT", [128, 4, 128], F32)
        b_cs = Buf("cossin")
        ang = self.alloc("ang", [128, 128], F32)
        ang2 = self.alloc("ang2", [128, 128], F32)
        ang3 = self.alloc("ang3", [128, 128], F32)
        C1 = 6.28125
        C2 = 2 * math.pi - C1
        b_ang = Buf("ang")
        qdec = self.alloc("qdec", [128, 8], F32)
        kdec = self.alloc("kdec", [128, 8], F32)
        b_dec = Buf("dec")
        maskc = self.alloc("maskc", [128, 8, 128], F32)
        b_mask = Buf("maskc")
        invf = self.alloc("invf", [128, 128], F32)
        b_invf = Buf("invf")
        posf = self.alloc("posf", [128, 16], F32)
        b_posf = Buf("posf")
        ii = self.alloc("iota_i", [128, 128], I32)
        b_ii = Buf("iota_i")
        ff = self.alloc("iota_f", [128, 128], F32)
        b_ff = Buf("iota_f")
        k.emit("pool", lambda e: e.iota(ii[:, 0:1], pattern=[[0, 1]], base=1, channel_multiplier=1), writes=[b_ii])
        k.emit("pool", lambda e: e.iota(ii[:, 1:2], pattern=[[0, 1]], base=127, channel_multiplier=-1),
               reads=[b_ii], writes=[b_ii])
        k.emit("dve", lambda e: e.tensor_copy(out=ff[:, 0:2], in_=ii[:, 0:2]), reads=[b_ii], writes=[b_ff])
        for h in range(8):
            self.act(qdec[:, h:h + 1], ff[:, 0:1], AF.Exp, reads=[b_ff], writes=[b_dec], scale=lg[h])
            self.act(kdec[:, h:h + 1], ff[:, 1:2], AF.Exp, reads=[b_ff], writes=[b_dec], scale=lg[h])
        self.ts(kdec[:, :], kdec[:, :], 0.0625, None, ALU.mult, None, reads=[b_dec], writes=[b_dec])
        for h in range(8):
            k.emit("pool", lambda e, h=h: e.memset(maskc[:, h, :], math.exp(-128.0 * lg[h])), reads=[b_mask], writes=[b_mask])
            k.emit("pool", lambda e, h=h: e.affine_select(out=maskc[:, h, :], in_=maskc[:, h, :], pattern=[[1, 128]],
                                                          compare_op=ALU.is_ge, fill=0.0, base=0, channel_multiplier=-1),
                   reads=[b_mask], writes=[b_mask])
        k.emit("pool", lambda e: e.iota(ii[:, :], pattern=[[1, 128]], base=0, channel_multiplier=0),
               reads=[b_ii, b_ff], writes=[b_ii])
        k.emit("dve", lambda e: e.tensor_copy(out=ff[:, :], in_=ii[:, :]), reads=[b_ii], writes=[b_ff])
        self.act(invf[:, :], ff[:, :], AF.Exp, reads=[b_ff], writes=[b_invf], scale=-math.log(10000.0) / 127.0)
        k.dma("sp", ii[0:16, :], self.pos.ap(), owner=b_ii, reads=[b_ii], writes=[b_ii])
        k.emit("dve", lambda e: e.tensor_copy(out=self.xt[0:16, 0:128], in_=ii[0:16, :]), reads=[b_ii], writes=[self.b_xt])
        ps, bps = self.bank()
        self.tr(ps[:, 0:16], bps, self.xt[0:16, 0:128], self.ident[0:16, 0:16], reads=[self.b_xt, self.b_ident])
        k.emit("dve", lambda e, ps=ps: e.tensor_copy(out=posf[:, :], in_=ps[:, 0:16]), reads=[bps], writes=[b_posf])
        k.emit("pool", lambda e: e.memset(S[:], 0.0), writes=[b_S])
        for h in range(8):
            k.dma("sp", self.st[:, h * 1024:(h + 1) * 1024], S[:].rearrange("p a b -> p (a b)"), owner=b_S,
                  reads=[b_S], writes=[self.b_st[h]])

        for pas in (1, 2):
            for tile in range(min(self.ntiles, T // 512)):
                chunks = [tile * NCH + i for i in range(NCH)]
                self.prenorm(src, bsrc, chunks, 2)
                self._cosT, self._sinT = cosT, sinT
                if True:
                    for ci, c in enumerate(chunks):
                        self.ts(ang[:], invf[:], posf[:, c:c + 1], None, ALU.mult, None,
                                reads=[b_invf, b_posf], writes=[b_ang])
                        for which, dstT in ((0, sinT), (1, cosT)):
                            if which == 1:
                                self.ts(ang[:], ang[:], math.pi / 2, None, ALU.add, None, reads=[b_ang], writes=[b_ang])
                            self.ts(ang2[:], ang[:], 1.0 / (2 * math.pi), None, ALU.mult, None, reads=[b_ang], writes=[b_ang])
                            k.emit("dve", lambda e: e.tensor_copy(out=ii[:, :], in_=ang2[:]), reads=[b_ang], writes=[b_ii])
                            k.emit("dve", lambda e: e.tensor_copy(out=ang2[:], in_=ii[:, :]), reads=[b_ii], writes=[b_ang])
                            self.stt(ang3[:], ang2[:], -C1, ang[:], ALU.mult, ALU.add, reads=[b_ang], writes=[b_ang])
                            self.stt(ang3[:], ang2[:], -C2, ang3[:], ALU.mult, ALU.add, reads=[b_ang], writes=[b_ang])
                            self.ts(ang2[:], ang3[:], math.pi, None, ALU.is_gt, None, reads=[b_ang], writes=[b_ang])
                            self.stt(ang3[:], ang2[:], -2 * math.pi, ang3[:], ALU.mult, ALU.add, reads=[b_ang], writes=[b_ang])
                            self.ts(ang2[:], ang3[:], -math.pi, None, ALU.is_lt, None, reads=[b_ang], writes=[b_ang])
                            self.stt(ang3[:], ang2[:], 2 * math.pi, ang3[:], ALU.mult, ALU.add, reads=[b_ang], writes=[b_ang])
                            self.act(dstT[:, ci, :], ang3[:], AF.Sin, reads=[b_ang], writes=[b_cs])
                for h in range(8):
                    cdec = math.exp(128.0 * lg[h])
                    slQK, bQK = self.slot()
                    if pas == 2:
                        self.load_w(slQK, bQK, self.r_w_in, 0, 16, h * 256, 256, 0)
                    self.load_w(slQK, bQK, self.r_w_in, 0, 16, 2048 + h * 256, 256, 256)
                    slV, bV = self.slot()
                    self.load_w(slV, bV, self.r_w_in, 0, 16, 4096 + h * 512, 512)
                    if pas == 2:
                        slG, bGs = self.slot()
                        self.load_w(slG, bGs, self.r_w_in, 0, 16, 8192 + h * 512, 512)
                    Sflat = S[:].rearrange("p a b -> p (a b)")
                    if pas == 2 and tile == 0 and RET_NOCC:
                        k.emit("pool", lambda e: e.memset(S[:], 0.0), reads=[b_S], writes=[b_S])
                    elif pas == 2 and tile == 0:
                        Stf = Stmp[:].rearrange("p a b -> p (a b)")
                        for r in range(NCORE):
                            k.dma("sp", Stf, self.st2[r * 128:(r + 1) * 128, h * 1024:(h + 1) * 1024], owner=b_Stmp,
                                  reads=[self.b_st2], writes=[b_Stmp])
                            if r == 0:
                                self.ts(Sflat, Stf, self.flag_sb[:, 0:1], None, ALU.mult, None,
                                        reads=[b_Stmp, self.b_flag], writes=[b_S])
                            else:
                                self.stt(Sflat, Stf, self.flag_sb[:, r:r + 1], Sflat, ALU.mult, ALU.add,
                                         reads=[b_Stmp, self.b_flag, b_S], writes=[b_S])
                    else:
                        k.dma("sp", Sflat, self.st[:, h * 1024:(h + 1) * 1024], owner=b_S,
                              reads=[self.b_st[h]], writes=[b_S])
                    if pas == 2:
                        self.act(Sb[:], S[:], AF.Copy, reads=[b_S], writes=[b_Sb])
                    for ci in range(NCH):
                        c0 = 0 if pas == 2 else 256
                        ps, bps = self.bank()
                        for kk in range(16):
                            self.mm(ps[:, c0:512], bps, self.hnT[:, kk, ci * 128:(ci + 1) * 128], slQK[:, kk, c0:512],
                                    start=(kk == 0), stop=(kk == 15), reads=[self.b_hnT, bQK])
                        if pas == 2:
                            self.ts(qks[:, 0:256], ps[:, 0:256], qdec[:, h:h + 1], None, ALU.mult, None,
                                    reads=[bps, b_dec], writes=[b_qks])
                        self.ts(qks[:, 256:512], ps[:, 256:512], kdec[:, h:h + 1], None, ALU.mult, None,
                                reads=[bps, b_dec], writes=[b_qks])
                        if pas == 2:
                            cs, sn = cosT[:, ci, :], sinT[:, ci, :]
                        else:
                            cs = sn = None
                        if pas == 1:
                            pass
                        for part in ((0, 1) if pas == 2 else (1,)):
                            base = part * 256
                            xe = qks[:, base:base + 256:2]
                            xo = qks[:, base + 1:base + 256:2]
                            self.tt(t1[:], xe, self.cs_ap(ci, 0), ALU.mult, reads=[b_qks, b_cs], writes=[b_t1])
                            self.tt(t2[:], xo, self.cs_ap(ci, 1), ALU.mult, reads=[b_qks, b_cs], writes=[b_t2])
                            self.tt(rq[:, base:base + 128], t1[:], t2[:], ALU.subtract, reads=[b_t1, b_t2], writes=[b_rq])
                            self.tt(t1[:], xo, self.cs_ap(ci, 0), ALU.mult, reads=[b_qks, b_cs], writes=[b_t1])
                            self.tt(t2[:], xe, self.cs_ap(ci, 1), ALU.mult, reads=[b_qks, b_cs], writes=[b_t2])
                            self.tt(rq[:, base + 128:base + 256], t1[:], t2[:], ALU.add, reads=[b_t1, b_t2], writes=[b_rq])
                        self.act(kdb[:], rq[:, 256:512], AF.Copy, reads=[b_rq], writes=[b_kdb])
                        pv, bpv = self.bank()
                        for kk in range(16):
                            self.mm(pv[:], bpv, self.hnT[:, kk, ci * 128:(ci + 1) * 128], slV[:, kk, :],
                                    start=(kk == 0), stop=(kk == 15), reads=[self.b_hnT, bV])
                        self.act(vb[:], pv[:], AF.Copy, reads=[bpv], writes=[b_vb])
                        if pas == 2:
                            pg, bpg = self.bank()
                            for kk in range(16):
                                self.mm(pg[:], bpg, self.hnT[:, kk, ci * 128:(ci + 1) * 128], slG[:, kk, :],
                                        start=(kk == 0), stop=(kk == 15), reads=[self.b_hnT, bGs])
                            self.act(sgt[:], pg[:], AF.Silu, reads=[bpg], writes=[b_sgt])
                            pt, bpt = self.bank()
                            for j in range(4):
                                self.tr(pt[:, j * 128:(j + 1) * 128], bpt, rq[:, j * 128:(j + 1) * 128], self.ident[:],
                                        reads=[b_rq, self.b_ident], inc=(j == 3))
                            k.emit("dve", lambda e, pt=pt: e.tensor_copy(out=qkT[:].rearrange("p a b -> p (a b)"), in_=pt[:]),
                                   reads=[bpt], writes=[b_qkT])
                            psc, bpsc = self.bank()
                            for eo in range(2):
                                self.mm(psc[:, 0:128], bpsc, qkT[:, 2 + eo, :], qkT[:, eo, :], start=(eo == 0), stop=(eo == 1),
                                        reads=[b_qkT])
                            self.tt(scm[:], psc[:, 0:128], maskc[:, h, :], ALU.mult, reads=[bpsc, b_mask], writes=[b_scm])
                            po, bpo = self.bank()
                            self.mm(po[:], bpo, scm[:], vb[:], start=True, stop=False, reads=[b_scm, b_vb], inc=False)
                            self.mm(po[:], bpo, qkT[:, 0, :], Sb[:, 0, :], start=False, stop=False, reads=[b_qkT, b_Sb], inc=False)
                            self.mm(po[:], bpo, qkT[:, 1, :], Sb[:, 1, :], start=False, stop=True, reads=[b_qkT, b_Sb])
                        for eo in range(2):
                            pd, bpd = self.bank()
                            self.mm(pd[:], bpd, kdb[:, eo * 128:(eo + 1) * 128], vb[:], start=True, stop=True,
                                    reads=[b_kdb, b_vb])
                            self.stt(S[:, eo, :], S[:, eo, :], cdec, pd[:], ALU.mult, ALU.add,
                                     reads=[b_S, bpd], writes=[b_S])
                        if pas == 2:
                            self.act(Sb[:], S[:], AF.Copy, reads=[b_S], writes=[b_Sb])
                            self.act(osb[:], po[:], AF.Copy, reads=[bpo], writes=[b_osb])
                            self.tt(self.scr[:, 0:512], osb[:], osb[:], ALU.mult, reads=[b_osb], writes=[self.b_scr])
                            self.red(self.sm[:, 1:2], self.scr[:, 0:512], reads=[self.b_scr], writes=[self.b_sm])
                            self.rstd_from_ss(1, 512)
                            self.stt(gt[:], osb[:], self.sm[:, 1:2], sgt[:], ALU.mult, ALU.mult,
                                     reads=[b_osb, self.b_sm, b_sgt], writes=[b_gt])
                            pt, bpt = self.bank()
                            for j in range(4):
                                self.tr(pt[:, j * 128:(j + 1) * 128], bpt, gt[:, j * 128:(j + 1) * 128], self.ident[:],
                                        reads=[b_gt, self.b_ident], inc=(j == 3))
                            k.emit("act", lambda e, pt=pt, h=h, ci=ci: e.activation(
                                out=gT[:, h * 4:(h + 1) * 4, ci * 128:(ci + 1) * 128],
                                in_=pt[:].rearrange("p (j t) -> p j t", j=4), func=AF.Copy),
                                reads=[bpt], writes=[b_gT])
                    k.dma("sp", self.st[:, h * 1024:(h + 1) * 1024], Sflat, owner=b_S, reads=[b_S], writes=[self.b_st[h]])
                if pas == 2:
                    self.outproj(gT, b_gT, 32, self.r_w_out, 5, src, bsrc, dst, bdst, chunks)
            if pas == 1 and not RET_NOCC:
                k.collective(self.st.ap().opt(), self.st2.ap().opt(), [list(range(NCORE))],
                             reads=self.b_st, writes=[self.b_st2])
        k.barrier()

    def cs_ap(self, ci, which):
        return (self._cosT if which == 0 else self._sinT)[:, ci, :]

    def dump(self, src):
        if not self.debug:
            return
        i = len(self.dumps)
        d = self.nc.dram_tensor("dump%d" % i, [T, D], F32, kind="ExternalOutput")
        self.dumps.append(d)
        b = Buf("dump%d" % i)
        for c in range(16):
            self.k.dma("sp", d[c * 128:(c + 1) * 128, :], src[c * 128:(c + 1) * 128, :], owner=b,
                       reads=[self.b_xs[c]], writes=[b])
        self.k.barrier()

    def build(self, stages=("gmlp", "ffn0", "ret", "ffn1")):
        k = self.k
        self.setup_common()
        self.gather_weights()
        cur, bcur = self.x_in, None
        last = stages[-1]
        for s in stages:
            dst, bdst = (self.out, self.b_out) if s == last else (self.xs, self.b_xs)
            if s == "gmlp":
                self.gmlp(cur, bcur, dst, bdst)
            elif s == "ffn0":
                self.ffn(0, cur, bcur, dst, bdst, 1, 3)
            elif s == "ret":
                self.retention(cur, bcur, dst, bdst)
            elif s == "ffn1":
                self.ffn(1, cur, bcur, dst, bdst, 3, 7)
            if s != last:
                self.dump(dst)
            cur, bcur = dst, bdst
        k.barrier()
        k.finish()
        return self.nc


_CACHE = {}


def make_in_maps(inputs):
    x = np.ascontiguousarray(inputs["x"], dtype=np.float32).reshape(NCORE, T, D)
    pos = np.ascontiguousarray(inputs["positions"]).astype(np.int32).reshape(NCORE, 16, 128)
    f32 = lambda a: np.ascontiguousarray(a, dtype=np.float32)
    norm_g = np.stack([inputs["mix_pre_g"][0], inputs["mix_post_g"][0], inputs["ffn_pre_g"][0], inputs["ffn_post_g"][0],
                       inputs["mix_pre_g"][1], inputs["mix_post_g"][1], inputs["ffn_pre_g"][1], inputs["ffn_post_g"][1]])
    shared = {
        "norm_g": f32(norm_g),
        "g_ln": f32(np.stack([inputs["gmlp_ln_g"][0], inputs["gmlp_ln_b"][0]])),
        "g_ws": f32(np.asarray(inputs["gmlp_w_s"][0]).reshape(8 * 128, 128)),
        "g_bs": f32(inputs["gmlp_b_s"][0]),
    }
    big = {
        "g_w_in": inputs["gmlp_w_in"][0], "g_w_out": inputs["gmlp_w_out"][0],
        "r_w_in": inputs["ret_w_in"][0], "r_w_out": inputs["ret_w_out"][0],
    }
    for i in range(2):
        shared["f_cw%d" % i] = f32(np.asarray(inputs["ffn_conv_w"][i]).reshape(3 * 88, 128))
        shared["f_cb%d" % i] = f32(np.asarray(inputs["ffn_conv_b"][i]).reshape(88, 128))
        big["f_w_up%d" % i] = inputs["ffn_w_up"][i]
        big["f_w_dn%d" % i] = inputs["ffn_w_down"][i]
    maps = []
    for c in range(NCORE):
        m = dict(shared)
        m["x"] = x[c]
        m["pos"] = pos[c]
        sel = np.zeros((128, 8), np.float32)
        if c % 2 == 1:
            sel[:, c - 1] = 1.0
        m["sel"] = sel
        for name, w in big.items():
            if SHARD_W:
                r = w.shape[0] // NCORE
                m[name] = f32(w[c * r:(c + 1) * r])
            else:
                m[name] = f32(w)
        maps.append(m)
    return maps


def kernel(**inputs):
    inputs = {k_: np.asarray(v) for k_, v in inputs.items()}
    if "nc" not in _CACHE:
        _CACHE["nc"] = Prog().build()
    nc = _CACHE["nc"]
    res = run_bass_kernel_spmd(nc, make_in_maps(inputs), core_ids=list(range(NCORE)))
    out = np.concatenate([np.asarray(r["out"]) for r in res.results], axis=0)
    return out.reshape(4, 4096, D).astype(np.float32)
```
